# Optimizing a Trainium2 kernel written in Bass

```python
import math
import jax, jax.numpy as jnp
from jax import lax
import numpy as np

D_MODEL = 1024
BATCH = 8
SEQ = 2048
DEPTH = 2

N_MEM = 256
HEAD_DIM = 64
MIX_WIDTH = 3 * D_MODEL // 4
MIX_HEADS = MIX_WIDTH // HEAD_DIM
MEM_WIDTH = D_MODEL - MIX_WIDTH
MEM_HEADS = 4
MEM_HEAD_DIM = MEM_WIDTH // MEM_HEADS
DECAY_LORA = 64
AAA_LORA = 64
GATE_LORA = 128
LORA_TOTAL = DECAY_LORA + AAA_LORA + GATE_LORA
RWKV_COLS = 3 * MIX_WIDTH + LORA_TOTAL
CHUNK = 128
GMLP_GROUPS = MIX_HEADS
D_FF = 11 * D_MODEL // 4
N_EXPERTS = 8
TOP_K = 2
N_A = (DEPTH + 1) // 2
N_B = DEPTH // 2
RMS_EPS = 1e-6
GN_EPS = 64e-5
LN_EPS = 1e-5

kernel_name = "hybrid_rwkv7_gmlp_memxattn_moe"


def rms_norm(x, g):
    xf = x.astype(jnp.float32)
    y = xf * lax.rsqrt(jnp.mean(xf * xf, axis=-1, keepdims=True) + RMS_EPS)
    return (y * g.astype(jnp.float32)).astype(x.dtype)


def token_shift_lerp(p, mu):
    prev = jnp.pad(p[:, :-1], ((0, 0), (1, 0), (0, 0)))
    return p + (prev - p) * mu


def rwkv7_time_mix(p, w0, w2, a0, a2, g2, k_k, k_a, r_k, lnx_w, lnx_b):
    p = p.astype(jnp.float32)
    b_, t_, _ = p.shape
    cuts = np.cumsum([MIX_WIDTH, MIX_WIDTH, MIX_WIDTH, DECAY_LORA, AAA_LORA]).tolist()
    r, k, v, wd, ad, gd = jnp.split(p, cuts, axis=-1)
    w = -jax.nn.softplus(-(w0 + jnp.tanh(wd) @ w2)) - 0.5
    decay = jnp.exp(-jnp.exp(w))
    a = jax.nn.sigmoid(a0 + ad @ a2)
    g = jax.nn.sigmoid(gd) @ g2

    def heads(t):
        return t.reshape(b_, t_, MIX_HEADS, HEAD_DIM)

    kk = heads(k * k_k)
    kk = kk / jnp.maximum(jnp.linalg.norm(kk, axis=-1, keepdims=True), 1e-12)
    k = k * (1.0 + (a - 1.0) * k_a)

    def seq_first(t):
        return jnp.moveaxis(t, 1, 0)

    def step(s, inp):
        r_t, w_t, k_t, v_t, kk_t, a_t = inp
        s_kk = jnp.einsum('bhvk,bhk->bhv', s, kk_t)
        s = (s * w_t[:, :, None, :]
             - s_kk[..., None] * (kk_t * a_t)[:, :, None, :]
             + v_t[..., None] * k_t[:, :, None, :])
        y_t = jnp.einsum('bhvk,bhk->bhv', s, r_t)
        return s, y_t

    s0 = jnp.zeros((b_, MIX_HEADS, HEAD_DIM, HEAD_DIM), jnp.float32)
    _, y = lax.scan(step, s0, (seq_first(heads(r)), seq_first(heads(decay)), seq_first(heads(k)),
                               seq_first(heads(v)), seq_first(kk), seq_first(heads(a))))
    y = jnp.moveaxis(y, 0, 1)
    mu = jnp.mean(y, axis=-1, keepdims=True)
    var = jnp.mean(jnp.square(y - mu), axis=-1, keepdims=True)
    y = ((y - mu) * lax.rsqrt(var + GN_EPS)).reshape(b_, t_, MIX_WIDTH) * lnx_w + lnx_b
    bonus = jnp.sum(heads(r) * heads(k) * r_k, axis=-1, keepdims=True) * heads(v)
    return (y + bonus.reshape(b_, t_, MIX_WIDTH)) * g


def chunked_gmlp(p, v_ln_g, v_ln_b, w_s, b_s):
    b_, t_, _ = p.shape
    h = jax.nn.gelu(p.astype(jnp.float32), approximate=False)
    u, v = jnp.split(h, 2, axis=-1)
    mu = jnp.mean(v, axis=-1, keepdims=True)
    var = jnp.mean(jnp.square(v - mu), axis=-1, keepdims=True)
    v = (v - mu) * lax.rsqrt(var + LN_EPS) * v_ln_g + v_ln_b
    v = v.reshape(b_, t_ // CHUNK, CHUNK, GMLP_GROUPS, MIX_WIDTH // GMLP_GROUPS)
    causal = jnp.tril(jnp.ones((CHUNK, CHUNK), jnp.float32))
    ws = w_s.astype(jnp.float32) * causal
    mixed = jnp.einsum('gts,bcsgd->bctgd', ws, v) + jnp.transpose(b_s)[None, None, :, :, None]
    return u * mixed.reshape(b_, t_, MIX_WIDTH)


def memory_attention(q, mem_n, w_kv):
    b_, t_, _ = q.shape
    k, v = jnp.split(mem_n @ w_kv, 2, axis=-1)
    q = q.reshape(b_, t_, MEM_HEADS, MEM_HEAD_DIM).astype(jnp.float32)
    k = k.reshape(b_, N_MEM, MEM_HEADS, MEM_HEAD_DIM).astype(jnp.float32)
    v = v.reshape(b_, N_MEM, MEM_HEADS, MEM_HEAD_DIM).astype(jnp.float32)
    s = jnp.einsum('bthd,bmhd->bhtm', q, k) * (1.0 / math.sqrt(MEM_HEAD_DIM))
    pr = jax.nn.softmax(s, axis=-1)
    o = jnp.einsum('bhtm,bmhd->bthd', pr, v)
    return o.reshape(b_, t_, MEM_WIDTH)


def swiglu(x, w_gu, w_down):
    gate, up = jnp.split(x @ w_gu, 2, axis=-1)
    return (jax.nn.silu(gate) * up) @ w_down


def moe_swiglu(x, w_router, w_gu, w_down):
    b_, t_, d_ = x.shape
    xt = x.reshape(b_ * t_, d_)
    logits = (xt @ w_router).astype(jnp.float32)
    top_v, top_i = lax.top_k(logits, TOP_K)
    gates = jax.nn.softmax(top_v, axis=-1)
    combine = jnp.sum(jax.nn.one_hot(top_i, N_EXPERTS, dtype=jnp.float32) * gates[..., None], axis=1)
    out = jnp.zeros((b_ * t_, d_), jnp.float32)
    for e in range(N_EXPERTS):
        out = out + combine[:, e:e + 1] * swiglu(xt, w_gu[e], w_down[e]).astype(jnp.float32)
    return out.reshape(b_, t_, d_).astype(x.dtype)


def setup_inputs(seed: int = 0) -> dict:
    key = jax.random.key(seed)
    ks = iter(jax.random.split(key, 40))
    nrm = lambda shape, scale: jax.random.normal(next(ks), shape, jnp.float32) * scale
    gain = lambda shape: 1.0 + nrm(shape, 0.02)
    d = D_MODEL
    return {
        "x": nrm((BATCH, SEQ, d), 1.0),
        "mem": nrm((BATCH, N_MEM, d), 1.0),
        "mem_norm_g": gain((d,)),
        "norm1_g": gain((DEPTH, d)),
        "w_kv_mem": nrm((DEPTH, d, 2 * MEM_WIDTH), d ** -0.5),
        "w_out": nrm((DEPTH, d, d), d ** -0.5),
        "norm2_g": gain((DEPTH, d)),
        "rwkv_w_in": nrm((N_A, d, RWKV_COLS + MEM_WIDTH), d ** -0.5),
        "rwkv_mu": jax.random.uniform(next(ks), (N_A, RWKV_COLS), jnp.float32, 0.0, 1.0),
        "rwkv_w0": jax.random.uniform(next(ks), (N_A, MIX_WIDTH), jnp.float32, -5.0, -0.5),
        "rwkv_w2": nrm((N_A, DECAY_LORA, MIX_WIDTH), 0.5 * DECAY_LORA ** -0.5),
        "rwkv_a0": nrm((N_A, MIX_WIDTH), 0.1),
        "rwkv_a2": nrm((N_A, AAA_LORA, MIX_WIDTH), AAA_LORA ** -0.5),
        "rwkv_g2": nrm((N_A, GATE_LORA, MIX_WIDTH), GATE_LORA ** -0.5),
        "rwkv_k_k": 0.85 + nrm((N_A, MIX_WIDTH), 0.05),
        "rwkv_k_a": 1.0 + nrm((N_A, MIX_WIDTH), 0.05),
        "rwkv_r_k": nrm((N_A, MIX_HEADS, HEAD_DIM), 0.1),
        "rwkv_lnx_w": gain((N_A, MIX_WIDTH)),
        "rwkv_lnx_b": nrm((N_A, MIX_WIDTH), 0.02),
        "ffn_w_gu": nrm((N_A, d, 2 * D_FF), d ** -0.5),
        "ffn_w_down": nrm((N_A, D_FF, d), D_FF ** -0.5),
        "gmlp_w_in": nrm((N_B, d, 2 * MIX_WIDTH + MEM_WIDTH), d ** -0.5),
        "gmlp_v_ln_g": gain((N_B, MIX_WIDTH)),
        "gmlp_v_ln_b": nrm((N_B, MIX_WIDTH), 0.02),
        "gmlp_w_s": nrm((N_B, GMLP_GROUPS, CHUNK, CHUNK), 0.5 * CHUNK ** -0.5),
        "gmlp_b_s": 1.0 + nrm((N_B, GMLP_GROUPS, CHUNK), 0.02),
        "moe_router": nrm((N_B, d, N_EXPERTS), d ** -0.5),
        "moe_w_gu": nrm((N_B, N_EXPERTS, d, 2 * D_FF), d ** -0.5),
        "moe_w_down": nrm((N_B, N_EXPERTS, D_FF, d), D_FF ** -0.5),
        "final_norm_g": gain((d,)),
    }


def reference(x, mem, mem_norm_g, norm1_g, w_kv_mem, w_out, norm2_g,
              rwkv_w_in, rwkv_mu, rwkv_w0, rwkv_w2, rwkv_a0, rwkv_a2, rwkv_g2,
              rwkv_k_k, rwkv_k_a, rwkv_r_k, rwkv_lnx_w, rwkv_lnx_b,
              ffn_w_gu, ffn_w_down,
              gmlp_w_in, gmlp_v_ln_g, gmlp_v_ln_b, gmlp_w_s, gmlp_b_s,
              moe_router, moe_w_gu, moe_w_down, final_norm_g):
    mem_n = rms_norm(mem, mem_norm_g)
    for i in range(DEPTH):
        j = i // 2
        n = rms_norm(x, norm1_g[i])
        if i % 2 == 0:
            p = n @ rwkv_w_in[j]
            p_mix, q = p[..., :RWKV_COLS], p[..., RWKV_COLS:]
            p_mix = token_shift_lerp(p_mix, rwkv_mu[j])
            y = rwkv7_time_mix(p_mix, rwkv_w0[j], rwkv_w2[j], rwkv_a0[j], rwkv_a2[j], rwkv_g2[j],
                               rwkv_k_k[j], rwkv_k_a[j], rwkv_r_k[j], rwkv_lnx_w[j], rwkv_lnx_b[j])
        else:
            p = n @ gmlp_w_in[j]
            p_mix, q = p[..., :2 * MIX_WIDTH], p[..., 2 * MIX_WIDTH:]
            y = chunked_gmlp(p_mix, gmlp_v_ln_g[j], gmlp_v_ln_b[j], gmlp_w_s[j], gmlp_b_s[j])
        o = memory_attention(q, mem_n, w_kv_mem[i])
        mixed = jnp.concatenate([y.astype(x.dtype), o.astype(x.dtype)], axis=-1)
        x = x + mixed @ w_out[i]
        n2 = rms_norm(x, norm2_g[i])
        if i % 2 == 0:
            x = x + swiglu(n2, ffn_w_gu[j], ffn_w_down[j])
        else:
            x = x + moe_swiglu(n2, moe_router[j], moe_w_gu[j], moe_w_down[j])
    return rms_norm(x, final_norm_g)
```

```python
import contextlib
import math
import numpy as np
import concourse.bass as bass
import concourse.mybir as mybir
from concourse.bass_utils import run_bass_kernel_spmd

F32 = mybir.dt.float32
BF16 = mybir.dt.bfloat16
AF = mybir.ActivationFunctionType
ALU = mybir.AluOpType
AX = mybir.AxisListType

T = 2048
D = 1024
NT = 16
NB = 256
NBLK = T // NB
MIX = 768
DFF = 2816
NHC = 22
C0 = math.exp(-0.5)
RMS_EPS = 1e-6
GN_EPS = 64e-5
LN_EPS = 1e-5


class Buf:
    __slots__ = ("name", "w", "r")

    def __init__(self, name):
        self.name = name
        self.w = None
        self.r = {}


class Lane:
    __slots__ = ("name", "sem")

    def __init__(self, name):
        self.name = name
        self.sem = None


class Op:
    __slots__ = ("idx", "eng", "fn", "lane", "deps", "signal", "sig", "order", "cost", "seg", "fin", "done", "pin")

    def __init__(self, idx, eng, fn, lane):
        self.idx = idx
        self.eng = eng
        self.fn = fn
        self.lane = lane
        self.deps = set()
        self.signal = False
        self.sig = None
        self.order = set()
        self.cost = 300.0
        self.seg = 0
        self.fin = 0.0
        self.done = False
        self.pin = False


ENGS = ("pe", "act", "dve", "pool", "sp")
import os as _os
PINS = set(_os.environ.get("PINS", "recip,scan,reduce,psum").split(","))


class Prog:
    def __init__(self, nc):
        self.nc = nc
        self.ops = []
        self.lanes = []
        self.nbuf = 0
        self.last = {}
        self.seg = 0
        self.cur_cost = None

    def buf(self, name=None, after=None):
        self.nbuf += 1
        b = Buf(name or f"b{self.nbuf}")
        if after:
            b.r = {(d.lane if d.lane is not None else d.eng): d for d in after}
        return b

    def lane(self, name=None):
        l = Lane(name or f"lane{len(self.lanes)}")
        self.lanes.append(l)
        return l

    def fence(self):
        self.seg += 1
        return list(self.last.values())

    def op(self, eng, fn, reads=(), writes=(), lane=None, cost=None):
        o = Op(len(self.ops), eng, fn, lane)
        o.seg = self.seg
        if cost is not None:
            o.cost = cost
        deps = set()
        order = set()
        key = lane if lane is not None else eng
        for b in reads:
            d = b.w
            if d is not None:
                if d.lane is None and lane is None and d.eng == eng and eng == "pe":
                    order.add(d)
                else:
                    deps.add(d)
            prev = b.r.get(key)
            if prev is not None:
                order.add(prev)
        for b in writes:
            for d in ([b.w] if b.w is not None else []) + list(b.r.values()):
                if d.lane is None and lane is None and d.eng == eng:
                    order.add(d)
                    continue
                if lane is not None and d.lane is lane:
                    order.add(d)
                    continue
                deps.add(d)
        deps.discard(o)
        order.discard(o)
        o.deps = deps
        o.order = order
        for d in deps:
            d.signal = True
        if lane is not None:
            o.signal = True
        for b in reads:
            b.r[lane if lane is not None else eng] = o
        for b in writes:
            b.w = o
            b.r = {}
        self.ops.append(o)
        self.last[lane if lane is not None else eng] = o
        return o

    def pe(self, fn, reads=(), writes=()):
        return self.op("pe", fn, reads, writes)

    def dma(self, q, lane, fn, reads=(), writes=()):
        return self.op(q, fn, reads, writes, lane=lane)

    def schedule(self, window=None, hop=200.0):
        import os
        if window is None:
            window = int(os.environ.get("SWIN", "40"))
        seng = os.environ.get("SENG")
        seng = set(seng.split(",")) if seng else None
        per = {e: [] for e in ENGS}
        if not os.environ.get("SCHED"):
            for o in self.ops:
                per[o.eng].append(o)
            return per
        free = {e: 0.0 for e in ENGS}
        segs = {}
        for o in self.ops:
            segs.setdefault(o.seg, []).append(o)
        for sg in sorted(segs):
            ops = segs[sg]
            pend = {e: [o for o in ops if o.eng == e] for e in ENGS}
            t0 = max(free.values())
            for e in ENGS:
                free[e] = t0
            remaining = len(ops)
            while remaining:
                best = None
                for e in ENGS:
                    lst = pend[e]
                    if not lst:
                        continue
                    fe = free[e]
                    n = 0
                    win_e = window if (seng is None or e in seng) else 1
                    for o in lst:
                        if n >= win_e or (o.pin and n > 0):
                            break
                        n += 1
                        ok = True
                        rdy = fe
                        for d in o.order:
                            if not d.done:
                                ok = False
                                break
                        if not ok:
                            continue
                        for d in o.deps:
                            if not d.done:
                                ok = False
                                break
                            t = d.fin + hop
                            if t > rdy:
                                rdy = t
                        if not ok:
                            continue
                        cand = (rdy, o.idx, o, e)
                        if best is None or cand[:2] < best[:2]:
                            best = cand
                        if rdy <= fe:
                            break
                rdy, _, o, e = best
                o.done = True
                if o.lane is not None:
                    o.fin = rdy + 2500.0 + o.cost
                    free[e] = rdy + 150.0
                else:
                    o.fin = rdy + o.cost
                    free[e] = o.fin
                pend[e].remove(o)
                per[e].append(o)
                remaining -= 1
        return per

    def emit(self, final_lanes=()):
        nc = self.nc
        with contextlib.ExitStack() as st:
            sems = {}
            for e in ("pe", "act", "dve", "pool"):
                sems[e] = st.enter_context(nc.semaphore("s_" + e))
            for l in self.lanes:
                l.sem = st.enter_context(nc.semaphore("l_" + l.name))
            cnt = {e: 0 for e in ENGS}
            lcnt = {}
            self.per = self.schedule()
            lastop = {}
            for e in ENGS:
                for o in self.per[e]:
                    lastop[(o.seg, o.lane if o.lane is not None else o.eng)] = o
            for o in self.ops:
                if any(d.seg < o.seg for d in o.deps):
                    nd = set()
                    for d in o.deps:
                        if d.seg < o.seg:
                            d = lastop[(d.seg, d.lane if d.lane is not None else d.eng)]
                            d.signal = True
                        nd.add(d)
                    o.deps = nd
            for o in [o for e in ENGS for o in self.per[e]]:
                if o.lane is not None:
                    c = lcnt.get(o.lane, 0) + 16
                    lcnt[o.lane] = c
                    o.sig = (o.lane.sem, c, 16)
                elif o.signal:
                    cnt[o.eng] += 1
                    o.sig = (sems[o.eng], cnt[o.eng], 1)
            block = st.enter_context(nc.Block())
            per = self.per
            stats = {}

            def run(e, h):
                waited = {}
                nw = 0
                for o in per[e]:
                    need = {}
                    for d in o.deps:
                        s, v, _ = d.sig
                        k = id(s)
                        if v > need.get(k, (None, 0))[1]:
                            need[k] = (s, v)
                    for k, (s, v) in need.items():
                        if waited.get(k, 0) >= v:
                            continue
                        h.wait_ge(s, v)
                        waited[k] = v
                        nw += 1
                    ins = o.fn(h)
                    if o.sig is not None:
                        ins.then_inc(o.sig[0], o.sig[2])
                if e == "sp":
                    for l in final_lanes:
                        if lcnt.get(l, 0) > 0:
                            h.wait_ge(l.sem, lcnt[l])
                stats[e] = (len(per[e]), nw)

            @block.tensor
            def _(h):
                run("pe", h)

            @block.scalar
            def _(h):
                run("act", h)

            @block.vector
            def _(h):
                run("dve", h)

            @block.gpsimd
            def _(h):
                run("pool", h)

            @block.sync
            def _(h):
                run("sp", h)

            self.stats = stats
            self.sigcnt = cnt


def drive(gens, window, preset=()):
    gens = list(gens)
    active = []
    nxt = 0
    locks = {}
    events = set(preset)
    pending = {}
    while nxt < len(gens) or active:
        while len(active) < window and nxt < len(gens):
            active.append(gens[nxt])
            nxt += 1
        progressed = False
        for g in list(active):
            while True:
                req = pending.get(id(g))
                if req is not None:
                    kind, name = req
                    if kind == "acq":
                        if locks.get(name) is None:
                            locks[name] = id(g)
                            pending[id(g)] = None
                        else:
                            break
                    elif kind == "wait":
                        if name in events:
                            pending[id(g)] = None
                        else:
                            break
                try:
                    r = next(g)
                    progressed = True
                except StopIteration:
                    active.remove(g)
                    progressed = True
                    break
                if r is None:
                    break
                kind, name = r
                if kind == "rel":
                    locks[name] = None
                elif kind == "set":
                    events.add(name)
                else:
                    pending[id(g)] = r
        assert progressed, "scheduler deadlock"


class Arena:
    def __init__(self, t, nwords):
        self.t = t
        self.n = nwords
        self.off = 0
        self.marks = []

    def alloc(self, shape, dtype=F32):
        nel = int(np.prod(shape))
        nby = nel * (2 if dtype == BF16 else 4)
        nw = (nby + 3) // 4
        nw = (nw + 1) // 2 * 2
        assert self.off + nw <= self.n, f"SBUF arena overflow: need {self.off + nw} have {self.n}"
        ap = self.t[:, self.off:self.off + nw]
        self.off += nw
        if dtype != F32:
            ap = ap.bitcast(dtype)
        ap = ap[:, 0:nel]
        if len(shape) == 2:
            ap = ap.rearrange("p (a b) -> p a b", a=shape[0])
        elif len(shape) == 3:
            ap = ap.rearrange("p (a b c) -> p a b c", a=shape[0], b=shape[1])
        elif len(shape) == 4:
            ap = ap.rearrange("p (a b c d) -> p a b c d", a=shape[0], b=shape[1], c=shape[2])
        return ap

    def view(self, off, shape, dtype=F32):
        save = self.off
        self.off = off
        ap = self.alloc(shape, dtype)
        self.off = save
        return ap

    def mark(self):
        self.marks.append(self.off)

    def release(self):
        self.off = self.marks.pop()


class K:
    def __init__(self, dbg=(), stage=99, nblk=NBLK, ncc=6, alg=99):
        self.dbg = set(dbg)
        self.stage = stage
        self.nblk = nblk
        self.ncc = ncc
        self.alg = alg
        import os
        self.skip = set(os.environ.get('SKIP', '').split(','))
        self.nc = nc = bass.Bass("TRN2", target_bir_lowering=False)
        self.P = Prog(nc)
        self.dbg_outs = {}
        self.dbg_lanes = []
        din = lambda n, s, d=F32: nc.dram_tensor(n, s, d, kind="ExternalInput").ap()
        self.x_d = din("x", [T, D])
        self.mem_d = din("mem", [256, D])
        self.grow_d = din("grows", [6, D])
        self.pc_d = din("pcols", [128, 64])
        self.win0_d = din("rwkv_w_in", [D, 2816])
        self.w2_d = din("rwkv_w2", [64, MIX])
        self.a2_d = din("rwkv_a2", [64, MIX])
        self.g2_d = din("rwkv_g2", [128, MIX])
        self.wkv_d = din("w_kv_mem", [2, D, 512])
        self.wout_d = din("w_out", [2, D, D])
        self.fgu_d = din("ffn_w_gu", [D, 2 * DFF])
        self.fdn_d = din("ffn_w_down", [DFF, D])
        self.win1_d = din("gmlp_w_in", [D, 1792])
        self.lnr_d = din("gmlp_ln", [2, MIX])
        self.wsT_d = din("gmlp_wsT", [128, 12, 128])
        self.bs_d = din("gmlp_bs", [12, 128])
        self.rt_d = din("moe_router", [D, 8])
        self.mgu_d = din("moe_w_gu", [8, D, 2 * DFF])
        self.mdn_d = din("moe_w_down", [8, DFF, D])
        self.y_d = nc.dram_tensor("y", [T, D], F32, kind="ExternalOutput").ap()
        self.mixT_d = nc.dram_tensor("mixT_scr", [D, T], BF16, kind="Internal").ap()

    def MM(self, out, lhsT, rhs, R, W, start=True, stop=True):
        n = rhs.free_size()
        self.P.op("pe", lambda h: h.matmul(out, lhsT, rhs, start=start, stop=stop), R, W, cost=max(64, n) / 1.9 + 8)

    def TR(self, out, in_, ident, R, W):
        self.P.op("pe", lambda h: h.transpose(out, in_, ident), R, W, cost=90.0)

    def ACT(self, out, in_, func, R, W, bias=None, scale=1.0, accum=None):
        kw = {}
        if bias is not None:
            kw["bias"] = bias
        if accum is not None:
            kw["accum_out"] = accum
        self.P.op("act", lambda h: h.activation(out, in_, func, scale=scale, **kw), R, W, cost=260 + out.free_size() / 1.2)

    def ecost(self, eng, out):
        n = out.free_size()
        return (120 + n / 0.96) if eng == "dve" else ((260 + n / 1.2) if eng == "act" else (200 + n / 0.45))

    def TT(self, eng, out, a, b, op, R, W):
        self.P.op(eng, lambda h: h.tensor_tensor(out, a, b, op), R, W, cost=self.ecost(eng, out))

    def TS(self, eng, out, a, s1, s2, op0, op1, R, W):
        if s2 is None:
            self.P.op(eng, lambda h: h.tensor_scalar(out, a, s1, None, op0), R, W, cost=self.ecost(eng, out))
        else:
            self.P.op(eng, lambda h: h.tensor_scalar(out, a, s1, s2, op0, op1), R, W, cost=self.ecost(eng, out))

    def STT(self, eng, out, a, s, b, op0, op1, R, W):
        self.P.op(eng, lambda h: h.scalar_tensor_tensor(out, a, s, b, op0, op1), R, W, cost=self.ecost(eng, out))

    def CP(self, eng, out, in_, R, W):
        if eng == "act":
            self.P.op(eng, lambda h: h.copy(out, in_), R, W, cost=self.ecost(eng, out))
        else:
            self.P.op(eng, lambda h: h.tensor_copy(out, in_), R, W, cost=self.ecost(eng, out))

    def MS(self, eng, out, val, W):
        self.P.op(eng, lambda h: h.memset(out, val), (), W)

    def RECIP(self, out, in_, R, W):
        o = self.P.op("dve", lambda h: h.reciprocal(out, in_), R, W, cost=120 + out.free_size() * 4.5)
        o.pin = "recip" in PINS

    def LOAD(self, q, lane, out, in_, W, R=()):
        o = self.P.dma(q, lane, lambda h: h.dma_start(out=out, in_=in_), R, W)
        o.cost = out.free_size() * 128 * 2 / 180.0

    def dump(self, name, ap, shape, R, dtype=F32):
        if name not in self.dbg:
            return
        d = self.nc.dram_tensor("dbg_" + name, shape, dtype, kind="ExternalOutput").ap()
        self.dbg_outs[name] = d
        l = self.P.lane("dbg_" + name)
        self.dbg_lanes.append(l)
        self.P.dma("sp", l, lambda h: h.dma_start(out=d, in_=ap), R, ())

    def build(self):
        nc, P = self.nc, self.P
        with contextlib.ExitStack() as st:
            NW = 52800
            big = st.enter_context(nc.sbuf_tensor("arena", [128, NW], F32))
            self.A = A = Arena(big, NW)
            self.ps = st.enter_context(nc.psum_tensor("ps", [128, 4096], F32))
            self.psb = [P.buf(f"psum{i}") for i in range(16)]
            self.out_lanes = []
            self.consts()
            self.mem_prep()
            if self.stage >= 1:
                self.layer0_mixer()
            if self.stage >= 2:
                self.resid_from_scratch(0)
            if self.stage >= 3:
                self.ffn_phase(0)
            if self.stage >= 4:
                self.layer1_mixer()
            if self.stage >= 5:
                self.ffn_phase(1)
            if self.stage >= 2:
                self.final_out()
            P.emit(final_lanes=self.out_lanes + self.dbg_lanes)
        return nc

    def bank(self, b):
        return self.ps[:, b * 512:(b + 1) * 512]

    def bankb(self, b):
        return [self.psb[2 * b], self.psb[2 * b + 1]]

    def consts(self):
        A, P = self.A, self.P
        self.identb = A.alloc([128], BF16)
        self.identf = A.alloc([128], F32)
        self.onesblk = A.alloc([128], BF16)
        self.blkf = A.alloc([128], F32)
        self.mAT = A.alloc([256], F32)
        self.mSL = A.alloc([128], F32)
        self.scanm = A.alloc([NB], F32)
        self.pc = A.alloc([64], F32)
        self.pc2 = A.alloc([32], F32)
        self.epsc = A.alloc([8], F32)
        self.bc = bc = P.buf("consts")
        l = self.lc = P.lane("const")
        self.MS("pool", self.identb, 0.0, [bc])
        P.op("pool", lambda h: h.affine_select(out=self.identb, in_=self.identb, pattern=[[-1, 128]],
                                               compare_op=ALU.not_equal, fill=1.0, base=0, channel_multiplier=1), [bc], [bc])
        self.CP("pool", self.identf, self.identb, [bc], [bc])
        self.MS("pool", self.onesblk, 0.0, [bc])
        self.MS("pool", self.onesblk[0:64, 0:64], 1.0, [bc])
        self.MS("pool", self.onesblk[64:128, 64:128], 1.0, [bc])
        self.CP("pool", self.blkf, self.onesblk, [bc], [bc])
        self.MS("pool", self.mAT, 0.0, [bc])
        P.op("pool", lambda h: h.affine_select(out=self.mAT[:, 0:128], in_=self.mAT[:, 0:128], pattern=[[-1, 128]],
                                               compare_op=ALU.is_ge, fill=1.0, base=0, channel_multiplier=1), [bc], [bc])
        P.op("pool", lambda h: h.affine_select(out=self.mAT[:, 128:256], in_=self.mAT[:, 128:256], pattern=[[-1, 128]],
                                               compare_op=ALU.is_gt, fill=1.0, base=0, channel_multiplier=1), [bc], [bc])
        self.MS("pool", self.mSL, 1.0, [bc])
        P.op("pool", lambda h: h.affine_select(out=self.mSL, in_=self.mSL, pattern=[[-1, 128]],
                                               compare_op=ALU.is_gt, fill=0.0, base=0, channel_multiplier=1), [bc], [bc])
        self.MS("pool", self.scanm, 1.0, [bc])
        self.MS("pool", self.scanm.rearrange("p (c t) -> p c t", t=128)[:, :, 0:1], 0.0, [bc])
        self.LOAD("sp", l, self.pc, self.pc_d[:, :], [bc])
        for i, v in enumerate([RMS_EPS, GN_EPS, LN_EPS, 1e-24, 0.0]):
            self.MS("pool", self.epsc[:, i:i + 1], v, [bc])
        self.TS("dve", self.pc2[:, 0:20], self.pc[:, 0:20], -1.0, 1.0, ALU.mult, ALU.add, [bc], [bc])
        self.TS("dve", self.pc2[:, 20:26], self.pc[:, 38:44], -1.0, 1.0, ALU.mult, ALU.add, [bc], [bc])

    def eps(self, i):
        return self.epsc[:, i:i + 1]

    def rms_tile(self, xs_ap, xs_b, gb_ap, gb_b, out_bf, out_b, sq_ap, st_ap, tmp_b, eng2="dve"):
        self.ACT(sq_ap, xs_ap, AF.Square, [xs_b], [tmp_b], accum=st_ap[:, 0:1])
        self.ACT(st_ap[:, 1:2], st_ap[:, 0:1], AF.Sqrt, [tmp_b], [tmp_b], bias=self.eps(0), scale=1.0 / D)
        self.RECIP(st_ap[:, 2:3], st_ap[:, 1:2], [tmp_b], [tmp_b])
        self.STT(eng2, out_bf, xs_ap, st_ap[:, 2:3], gb_ap, ALU.mult, ALU.mult, [xs_b, gb_b, tmp_b], [out_b])

    def mem_prep(self):
        A, P = self.A, self.P
        self.memT = A.alloc([8, 256], BF16)
        self.memT_b = P.buf("memT")
        A.mark()
        gb = A.alloc([D], F32)
        xs = A.alloc([D], F32)
        xn = A.alloc([D], BF16)
        sq = A.alloc([D], BF16)
        stt = A.alloc([4], F32)
        b_gb, b_xs, b_xn, b_t = P.buf(), P.buf(), P.buf(), P.buf()
        l1, l2 = P.lane("memg"), P.lane("memx")
        self.LOAD("sp", l1, gb, self.grow_d[0, :].partition_broadcast(128), [b_gb])
        for mt in range(2):
            self.LOAD("sp", l2, xs, self.mem_d[mt * 128:(mt + 1) * 128, :], [b_xs])
            self.rms_tile(xs, b_xs, gb, b_gb, xn, b_xn, sq, stt, b_t)
            pb = self.bank(mt).bitcast(BF16)
            for c in range(8):
                self.TR(pb[:, c * 128:(c + 1) * 128], xn[:, c * 128:(c + 1) * 128], self.identb, [b_xn, self.bc], self.bankb(mt))
            self.CP("act", self.memT[:, :, mt * 128:(mt + 1) * 128], pb.rearrange("p (c t) -> p c t", c=8), self.bankb(mt), [self.memT_b])
        self.dump("memT", self.memT, [128, 8, 256], [self.memT_b], BF16)
        self.mem_fence = P.fence()
        A.release()

    def kv_prep(self, li, kT, Vm, kv_b, wkv, wkv_b, lane):
        self.LOAD("pool", lane, wkv, self.wkv_d[li].rearrange("(c p) n -> p c n", p=128), [wkv_b])
        self.MS("pool", kT, 0.0, [kv_b])
        for j in range(2):
            pb = self.bank(j)
            for c in range(8):
                self.MM(pb[:, 0:256], wkv[:, c, j * 128:(j + 1) * 128], self.memT[:, c, :], [wkv_b, self.memT_b], self.bankb(j), start=(c == 0), stop=(c == 7))
            for hh in range(2):
                self.CP("act", kT[hh * 64:(hh + 1) * 64, 2 * j + hh, :], pb[hh * 64:(hh + 1) * 64, 0:256], self.bankb(j), [kv_b])
        for mc in range(2):
            pb = self.bank(2 + mc)
            for c in range(8):
                self.MM(pb[:, 0:256], self.memT[:, c, mc * 128:(mc + 1) * 128], wkv[:, c, 256:512], [wkv_b, self.memT_b], self.bankb(2 + mc), start=(c == 0), stop=(c == 7))
            self.CP("dve", Vm[:, mc, :], pb[:, 0:256], self.bankb(2 + mc), [kv_b])

    def attention_tile(self, qT, q_b, tsl, kT, Vm, kv_b, mixT_out, mix_b, pbanks, S):
        b0, b1 = pbanks
        sc = [self.bank(b0), self.bank(b1)]
        for h in range(4):
            j, hp = h // 2, (h % 2) * 64
            if ("odd" in self.skip and h % 2 == 1) or ("even" in self.skip and h % 2 == 0):
                continue
            self.MM(sc[j][:, (h % 2) * 256:(h % 2 + 1) * 256], qT[:, j, tsl], kT[:, h, :], [q_b, kv_b], self.bankb(pbanks[j]))
        mx, scs, pr, sm, prn, prT, tb = S["mx"], S["scs"], S["pr"], S["sm"], S["pr"], S["prT"], S["b"]
        if "att1" in self.skip:
            return
        for j in range(2):
            v3 = sc[j].rearrange("p (h m) -> p h m", h=2)
            self.P.op("dve", lambda h, v3=v3, j=j: h.tensor_reduce(mx[:, 2 * j:2 * j + 2], v3, AX.X, ALU.max), self.bankb(pbanks[j]), [tb]).pin = "reduce" in PINS
            self.TT("dve", scs[:, 2 * j:2 * j + 2, :], v3, mx[:, 2 * j:2 * j + 2].unsqueeze(2).to_broadcast([128, 2, 256]), ALU.subtract, self.bankb(pbanks[j]) + [tb], [tb])
        if "att2" in self.skip:
            return
        self.ACT(pr, scs, AF.Exp, [tb], [tb], scale=0.125)
        self.P.op("dve", lambda h: h.tensor_reduce(sm, pr, AX.X, ALU.add), [tb], [tb]).pin = "reduce" in PINS
        self.RECIP(sm, sm, [tb], [tb])
        self.TT("dve", prn, pr, sm.unsqueeze(2).to_broadcast([128, 4, 256]), ALU.mult, [tb], [tb])
        if "att3" in self.skip:
            return
        pbT = self.bank(b0).bitcast(BF16)
        for h in range(4):
            for mc in range(2):
                self.TR(pbT[:, (h * 2 + mc) * 128:(h * 2 + mc + 1) * 128], prn[:, h, mc * 128:(mc + 1) * 128], self.identb, [tb, self.bc], self.bankb(b0))
        self.CP("act", prT, pbT.rearrange("p (a t) -> p a t", a=8), self.bankb(b0), [tb])
        if "att4" in self.skip:
            return
        po = self.bank(b1)
        for h in range(4):
            j, hp = h // 2, (h % 2) * 64
            for mc in range(2):
                self.MM(po[hp:hp + 64, j * 128:(j + 1) * 128], Vm[:, mc, h * 64:(h + 1) * 64], prT[:, h * 2 + mc, :], [kv_b, tb], self.bankb(b1), start=(mc == 0), stop=(mc == 1))
        self.CP("dve", mixT_out, po[:, 0:256].rearrange("p (j t) -> p j t", j=2), self.bankb(b1), [mix_b])

    def attn_scratch(self):
        A = self.A
        return dict(mx=A.alloc([4], F32), scs=A.alloc([4, 256], F32), pr=A.alloc([4, 256], BF16), sm=A.alloc([4], F32),
                    prT=A.alloc([8, 128], BF16), b=self.P.buf("attn_s"))

    def layer0_mixer(self):
        A, P = self.A, self.P
        A.mark()
        fence0 = self.mem_fence
        nb = lambda n=None: P.buf(n, after=fence0)
        pc, pc2 = self.pc, self.pc2
        kT = A.alloc([4, 256], BF16); Vm = A.alloc([2, 256], BF16); b_kv = nb("kv")
        A.mark()
        wkv = A.alloc([8, 512], BF16); b_wkv = nb("wkv")
        lkv = P.lane("wkv0")
        self.kv_prep(0, kT, Vm, b_kv, wkv, b_wkv, lkv)
        A.release()
        fence0 = P.fence()
        win = A.alloc([8, 2816], BF16); b_win = nb("win")
        w2z = A.alloc([MIX], BF16)
        a2z = A.alloc([MIX], BF16)
        g2 = A.alloc([MIX], BF16); b_lora = nb("lora")
        gb = A.alloc([D], F32); b_gb = nb("gb")
        lw = [P.lane(f"w0_{i}") for i in range(3)]
        self.LOAD("sp", lw[0], gb, self.grow_d[1, :].partition_broadcast(128), [b_gb])
        for c in range(8):
            self.LOAD("pool", lw[1], win[:, c, :], self.win0_d[c * 128:(c + 1) * 128, :], [b_win])
        self.MS("pool", w2z[64:128, :], 0.0, [b_lora])
        self.MS("pool", a2z[0:64, :], 0.0, [b_lora])
        self.LOAD("pool", lw[2], w2z[0:64, :], self.w2_d[:, :], [b_lora])
        self.LOAD("pool", lw[2], a2z[64:128, :], self.a2_d[:, :], [b_lora])
        self.LOAD("pool", lw[2], g2, self.g2_d[:, :], [b_lora])
        xs1 = A.alloc([D], F32); xs = [xs1, xs1]; bxs1 = nb(); b_xs = [bxs1, bxs1]
        xn1 = A.alloc([D], BF16); xn = [xn1, xn1]; bxn1 = nb(); b_xn = [bxn1, bxn1]
        sqj = A.alloc([D], BF16); stt = [A.alloc([4], F32) for _ in range(2)]; b_nt = [nb() for _ in range(2)]
        lx1 = P.lane("x0_0"); lx = [lx1, lx1]
        nT = [A.alloc([8, NB + 1], BF16) for _ in range(2)]; b_nT = [nb() for _ in range(2)]
        pm = A.alloc([20, NB], F32); b_pm = [nb(f"pm{i}") for i in range(20)]
        ltmp = [A.alloc([NB], F32) for _ in range(3)]; b_ltmp = [nb() for _ in range(3)]
        qT = [A.alloc([2, NB], BF16) for _ in range(2)]; b_qT = [nb() for _ in range(2)]
        shp = [A.alloc([2, NB], BF16) for _ in range(2)]; b_shp = [nb() for _ in range(2)]
        mixT = [A.alloc([8, NB], BF16) for _ in range(2)]; b_mix = [[nb() for _ in range(8)] for _ in range(2)]
        lmix = [P.lane(f"mix{i}") for i in range(2)]
        AS = self.attn_scratch()
        NF = 12
        ft = [A.alloc([NB], F32) for _ in range(NF)]; b_ft = [nb() for _ in range(NF)]
        bt = [A.alloc([NB], BF16) for _ in range(2)]; b_bt = [nb() for _ in range(2)]
        def slot_tiles():
            d = {}
            d["gate"] = A.alloc([NB], F32); d["bonus"] = A.alloc([NB], F32); d["gC"] = A.alloc([2], F32)
            d["Kq"] = A.alloc([NB], BF16); d["Bq"] = A.alloc([NB], BF16); d["BgC"] = A.alloc([NB], BF16)
            d["KgC"] = A.alloc([NB], BF16); d["Vb"] = A.alloc([NB], BF16); d["KRz"] = A.alloc([2, 2, 2, 128], BF16); d["Kp"] = A.alloc([NB], BF16)
            d["RgF"] = A.alloc([NB], F32)
            d["tok"] = A.alloc([8, 128], BF16)
            oAT = [A.off, A.off + 512]
            d["AT1"] = A.alloc([4, 256], BF16); d["AT2"] = A.alloc([4, 256], BF16); d["Aab"] = A.alloc([4, 128], BF16)
            oX = [A.off, A.off + 256]; d["X"] = [A.alloc([4, 128], BF16) for _ in range(2)]
            oXT = [A.off, A.off + 256]; d["XT"] = [A.alloc([4, 128], BF16) for _ in range(2)]
            d["PT"] = [A.alloc([4, 128], BF16) for _ in range(2)]
            d["Nc"] = A.alloc([2, 64], F32)
            d["b_in"] = nb(); self.MS("pool", d["KRz"], 0.0, [d["b_in"]]); d["b_tok"] = nb(); d["b_A"] = nb(); d["b_X"] = [nb(), nb()]; d["b_XT"] = [nb(), nb()]; d["b_PT"] = [nb(), nb()]
            d["AV"] = A.view(oX[0], [4, 64], BF16); d["b_AV"] = d["b_X"][0]
            d["yb"] = A.view(oX[1], [NB], BF16)
            d["WU"] = A.view(oXT[0], [2, 2, 2, 64], BF16); d["b_WU"] = d["b_XT"][0]
            d["McT"] = A.view(oXT[1], [2, 128], BF16); d["QcT"] = A.view(oXT[1] + 128, [2, 128], BF16)
            d["b_MN"] = d["b_XT"][1]; d["b_Q"] = d["b_XT"][1]
            d["b_Nc"] = nb()
            d["y"] = A.view(oAT[0], [NB], F32); d["yc"] = A.view(oAT[0] + 256, [NB], F32); d["rs"] = A.view(oAT[1], [NB], F32)
            d["b_y"] = d["b_A"]
            return d
        NSLOT = 3
        SL = [slot_tiles() for _ in range(NSLOT)]
        Sf = A.alloc([6, 64], F32); Sb = A.alloc([6, 64], BF16); Sbd = A.alloc([6, 128], BF16); b_S = [nb(f"S{i}") for i in range(6)]
        identb4 = self.identb.unsqueeze(1).to_broadcast([128, 4, 128])
        for cc in range(6):
            self.MS("pool", Sf[:, cc, :], 0.0, [b_S[cc]])
            self.MS("pool", Sb[:, cc, :], 0.0, [b_S[cc]])
            self.MS("pool", Sbd[:, cc, :], 0.0, [b_S[cc]])

        psum_rot = {"f": [0, 1], "u0": [2, 3], "u1": [4, 5], "u2": [6, 7]}
        rot_idx = {"f": 0, "u0": 0, "u1": 0, "u2": 0}

        def nextbank(key):
            lst = psum_rot[key]
            b = lst[rot_idx[key] % len(lst)]
            rot_idx[key] += 1
            return b

        def front(bi):
            s = bi % 2
            t0 = bi * NB
            for tt in range(2):
                k = tt
                self.LOAD("sp", lx[k], xs[k], self.x_d[t0 + tt * 128:t0 + (tt + 1) * 128, :], [b_xs[k]])
                self.rms_tile(xs[k], b_xs[k], gb, b_gb, xn[k], b_xn[k], sqj, stt[k], b_nt[k])
                b = nextbank("f")
                pb = self.bank(b).bitcast(BF16)
                for c in range(8):
                    self.TR(pb[:, c * 128:(c + 1) * 128], xn[k][:, c * 128:(c + 1) * 128], self.identb, [b_xn[k], self.bc], self.bankb(b))
                self.CP("act" if tt == 0 else "dve", nT[s][:, :, 1 + tt * 128:1 + (tt + 1) * 128], pb.rearrange("p (c t) -> p c t", c=8), self.bankb(b), [b_nT[s]])
                yield
            yield ("wait", f"pmfree{bi}")
            if bi == 0:
                self.MS("pool", nT[s][:, :, 0:1], 0.0, [b_nT[s]])
            else:
                self.CP("pool", nT[s][:, :, 0:1], nT[1 - s][:, :, NB:NB + 1], [b_nT[1 - s]], [b_nT[s]])
            for fc in range(22):
                if "inproj" in self.skip:
                    continue
                b = nextbank("f")
                pb = self.bank(b)
                if fc < 20:
                    for c in range(8):
                        self.MM(pb[:, 0:NB + 1], win[:, c, fc * 128:(fc + 1) * 128], nT[s][:, c, 0:NB + 1], [b_win, b_nT[s]], self.bankb(b), start=(c == 0), stop=(c == 7))
                    k = fc % 3
                    self.ACT(ltmp[k], pb[:, 0:NB], AF.Copy, self.bankb(b) + [self.bc], [b_ltmp[k]], scale=pc[:, fc:fc + 1])
                    self.STT("dve", pm[:, fc, :], pb[:, 1:NB + 1], pc2[:, fc:fc + 1], ltmp[k], ALU.mult, ALU.add, self.bankb(b) + [b_ltmp[k], self.bc], [b_pm[fc]])
                else:
                    j = fc - 20
                    for c in range(8):
                        self.MM(pb[:, 0:NB], win[:, c, fc * 128:(fc + 1) * 128], nT[s][:, c, 1:NB + 1], [b_win, b_nT[s]], self.bankb(b), start=(c == 0), stop=(c == 7))
                    self.CP("act", qT[s][:, j, :], pb[:, 0:NB], self.bankb(b), [b_qT[s]])
                yield
            if "shp" in self.skip:
                return
            self.ACT(shp[s][0:64, 0, :], pm[0:64, 18, :], AF.Tanh, [b_pm[18]], [b_shp[s]])
            self.CP("pool", shp[s][64:128, 0, :], pm[64:128, 18, :], [b_pm[18]], [b_shp[s]])
            self.ACT(shp[s][:, 1, :], pm[:, 19, :], AF.Sigmoid, [b_pm[19]], [b_shp[s]])
            yield

        def prep(bi, cc, sl):
            s = bi % 2
            d = SL[sl]
            key = f"u{sl}"
            r, k, v = pm[:, cc, :], pm[:, 6 + cc, :], pm[:, 12 + cc, :]
            br, bk, bv = b_pm[cc], b_pm[6 + cc], b_pm[12 + cc]
            cs_ = slice(cc * 128, (cc + 1) * 128)
            col = lambda base: pc[:, base + cc:base + cc + 1]
            f_lw, f_cs, f_csp, f_g, f_gi, f_gp, f_a, f_kk, f_rn, f_kn, f_b, f_gq, f_t = range(13)
            F = lambda i: ft[i]
            Bf = lambda i: b_ft[i]
            b1 = nextbank(key); p1 = self.bank(b1)
            self.MM(p1[:, 0:NB], w2z[:, cs_], shp[s][:, 0, :], [b_lora, b_shp[s]], [self.psb[2 * b1]])
            self.MM(p1[:, NB:2 * NB], a2z[:, cs_], shp[s][:, 0, :], [b_lora, b_shp[s]], [self.psb[2 * b1 + 1]])
            b2 = nextbank(key); p2 = self.bank(b2)
            self.MM(p2[:, 0:NB], g2[:, cs_], shp[s][:, 1, :], [b_lora, b_shp[s]], [self.psb[2 * b2]])
            self.ACT(F(f_lw), p1[:, 0:NB], AF.Sigmoid, [self.psb[2 * b1], self.bc], [Bf(f_lw)], bias=col(20))
            self.ACT(F(f_a), p1[:, NB:2 * NB], AF.Sigmoid, [self.psb[2 * b1 + 1], self.bc], [Bf(f_a)], bias=col(26))
            self.CP("act", d["gate"], p2[:, 0:NB], [self.psb[2 * b2]], [d["b_in"]])
            yield
            o_ = self.P.op("dve", lambda h: h.tensor_tensor_scan(F(f_cs), self.scanm, F(f_lw), 0.0, ALU.mult, ALU.add), [Bf(f_lw), self.bc], [Bf(f_cs)])
            o_.pin = "scan" in PINS
            self.TT("dve", F(f_csp), F(f_cs), F(f_lw), ALU.subtract, [Bf(f_cs), Bf(f_lw)], [Bf(f_csp)])
            self.ACT(F(f_g), F(f_cs), AF.Exp, [Bf(f_cs)], [Bf(f_g)], scale=-C0)
            self.ACT(F(f_gi), F(f_cs), AF.Exp, [Bf(f_cs)], [Bf(f_gi)], scale=C0)
            self.ACT(F(f_gp), F(f_csp), AF.Exp, [Bf(f_csp)], [Bf(f_gp)], scale=-C0)
            self.TS("dve", F(f_kk), k, col(32), None, ALU.mult, None, [bk, self.bc], [Bf(f_kk)])
            self.ACT(bt[0], F(f_kk), AF.Square, [Bf(f_kk)], [b_bt[0]])
            self.MM(p2[:, NB:2 * NB], self.onesblk, bt[0], [self.bc, b_bt[0]], [self.psb[2 * b2 + 1]])
            self.ACT(F(f_rn), p2[:, NB:2 * NB], AF.Sqrt, [self.psb[2 * b2 + 1], self.bc], [Bf(f_rn)], bias=self.eps(3))
            self.RECIP(F(f_rn), F(f_rn), [Bf(f_rn)], [Bf(f_rn)])
            self.TT("dve", F(f_kk), F(f_kk), F(f_rn), ALU.mult, [Bf(f_kk), Bf(f_rn)], [Bf(f_kk)])
            yield
            self.TS("dve", F(f_kn), F(f_a), col(38), pc2[:, 20 + cc:21 + cc], ALU.mult, ALU.add, [Bf(f_a), self.bc], [Bf(f_kn)])
            self.TT("dve", F(f_kn), F(f_kn), k, ALU.mult, [Bf(f_kn), bk], [Bf(f_kn)])
            self.TT("pool", F(f_b), F(f_kk), F(f_a), ALU.mult, [Bf(f_kk), Bf(f_a)], [Bf(f_b)])
            self.STT("dve", bt[1], r, col(44), F(f_kn), ALU.mult, ALU.mult, [br, Bf(f_kn), self.bc], [b_bt[1]])
            b3 = nextbank(key); p3 = self.bank(b3)
            self.MM(p3[:, 0:NB], self.onesblk, bt[1], [self.bc, b_bt[1]], [self.psb[2 * b3]])
            self.TT("dve", d["bonus"], p3[:, 0:NB], v, ALU.mult, [self.psb[2 * b3], bv], [d["b_in"]])
            yield
            KRz = d["KRz"]
            c2 = lambda ap: ap.rearrange("p (c t) -> p c t", c=2)
            self.TT("pool", d["Kp"], F(f_kk), F(f_gp), ALU.mult, [Bf(f_kk), Bf(f_gp)], [d["b_in"]])
            for hh in range(2):
                hsl = slice(hh * 64, (hh + 1) * 64)
                self.TT("pool", KRz[hsl, hh, :, 0, :], c2(F(f_kk)[hsl]), c2(F(f_gp)[hsl]), ALU.mult, [Bf(f_kk), Bf(f_gp)], [d["b_in"]])
                self.TT("pool", KRz[hsl, hh, :, 1, :], c2(r[hsl]), c2(F(f_g)[hsl]), ALU.mult, [br, Bf(f_g)], [d["b_in"]])
            for ch in range(2):
                tsl = slice(ch * 128, (ch + 1) * 128)
                self.TS("dve", F(f_gq)[:, tsl], F(f_gi)[:, tsl], F(f_g)[:, ch * 128 + 127:ch * 128 + 128], None, ALU.mult, None, [Bf(f_gi), Bf(f_g)], [Bf(f_gq)])
                self.CP("pool", d["gC"][:, ch:ch + 1], F(f_g)[:, ch * 128 + 127:ch * 128 + 128], [Bf(f_g)], [d["b_in"]])
            self.TT("dve", d["RgF"], r, F(f_g), ALU.mult, [br, Bf(f_g)], [d["b_in"]])
            self.TT("dve", d["Kq"], F(f_kn), F(f_gi), ALU.mult, [Bf(f_kn), Bf(f_gi)], [d["b_in"]])
            self.TT("pool", d["Bq"], F(f_b), F(f_gi), ALU.mult, [Bf(f_b), Bf(f_gi)], [d["b_in"]])
            self.TT("dve", d["KgC"], F(f_kn), F(f_gq), ALU.mult, [Bf(f_kn), Bf(f_gq)], [d["b_in"]])
            self.TT("pool", d["BgC"], F(f_b), F(f_gq), ALU.mult, [Bf(f_b), Bf(f_gq)], [d["b_in"]])
            self.CP("act", d["Vb"], v, [bv], [d["b_in"]])
            yield

        def algo(bi, cc, sl):
            s = bi % 2
            d = SL[sl]
            key = f"u{sl}"
            bin_ = d["b_in"]
            KRz, Kq, Bq, tok = d["KRz"], d["Kq"], d["Bq"], d["tok"]
            hs = lambda hh: slice(hh * 64, (hh + 1) * 64)
            b = nextbank(key); pb = self.bank(b).bitcast(BF16)
            for ch in range(2):
                tsl = slice(ch * 128, (ch + 1) * 128)
                srcs = [d["Vb"][:, tsl], d["Kp"][:, tsl], d["BgC"][:, tsl], d["KgC"][:, tsl]]
                for q in range(4):
                    i = ch * 4 + q
                    self.TR(pb[:, i * 128:(i + 1) * 128], srcs[q], self.identb, [bin_, self.bc], self.bankb(b))
            self.CP("act", tok, pb.rearrange("p (a t) -> p a t", a=8), self.bankb(b), [d["b_tok"]])
            yield
            mATb = self.mAT.unsqueeze(1).to_broadcast([128, 2, 256])
            for which, L, dst in ((0, Bq, d["AT1"]), (1, Kq, d["AT2"])):
                for ch in range(2):
                    b = nextbank(key); pb = self.bank(b)
                    for hh in range(2):
                        self.MM(pb[:, hh * 256:(hh + 1) * 256], L[:, ch * 128:(ch + 1) * 128], KRz[:, hh, ch].rearrange("p a t -> p (a t)"), [bin_], self.bankb(b))
                    self.TT("dve", dst[:, ch * 2:ch * 2 + 2, :], pb.rearrange("p (u x) -> p u x", u=2), mATb, ALU.mult, self.bankb(b) + [self.bc], [d["b_A"]])
                yield
            b = nextbank(key); pb = self.bank(b)
            for ch in range(2):
                for hh in range(2):
                    u = ch * 2 + hh
                    self.MM(pb[:, u * 128:(u + 1) * 128], KRz[:, hh, ch, 0, :], Bq[:, ch * 128:(ch + 1) * 128], [bin_], self.bankb(b))
            self.TT("dve", d["Aab"], pb.rearrange("p (u x) -> p u x", u=4), self.mSL.unsqueeze(1).to_broadcast([128, 4, 128]), ALU.mult, self.bankb(b) + [self.bc], [d["b_A"]])
            AabT = d["AT1"][:, :, 0:128]
            ArbT = d["AT1"][:, :, 128:256]
            AakT = d["AT2"][:, :, 0:128]
            ArkT = d["AT2"][:, :, 128:256]
            self.TT("pool", d["PT"][0], identb4, AabT, ALU.subtract, [self.bc, d["b_A"]], [d["b_PT"][0]])
            yield
            Xc, XTc, bX, bXT = d["Aab"], AabT, d["b_A"], d["b_A"]
            for lv in range(6):
                Xn, bXn = d["X"][lv % 2], d["b_X"][lv % 2]
                XTn, bXTn = d["XT"][lv % 2], d["b_XT"][lv % 2]
                b = nextbank(key); pb = self.bank(b)
                for u in range(4):
                    self.MM(pb[:, u * 128:(u + 1) * 128], XTc[:, u, :], Xc[:, u, :], [bX, bXT], self.bankb(b))
                self.CP("act", Xn, pb.rearrange("p (u x) -> p u x", u=4), self.bankb(b), [bXn])
                if lv < 5:
                    b = nextbank(key); pb2 = self.bank(b)
                    for u in range(4):
                        self.MM(pb2[:, u * 128:(u + 1) * 128], Xc[:, u, :], XTc[:, u, :], [bX, bXT], self.bankb(b))
                    self.CP("dve", XTn, pb2.rearrange("p (u x) -> p u x", u=4), self.bankb(b), [bXTn])
                yield
                PTo, bPo = d["PT"][lv % 2], d["b_PT"][lv % 2]
                PTn, bPn = d["PT"][(lv + 1) % 2], d["b_PT"][(lv + 1) % 2]
                b = nextbank(key); pb3 = self.bank(b)
                for u in range(4):
                    self.MM(pb3[:, u * 128:(u + 1) * 128], Xn[:, u, :], PTo[:, u, :], [bXn, bPo], self.bankb(b))
                self.TT("dve", PTn, pb3.rearrange("p (u x) -> p u x", u=4), PTo, ALU.add, self.bankb(b) + [bPo], [bPn])
                Xc, XTc, bX, bXT = Xn, XTn, bXn, bXTn
                yield
            TinvT, bT = d["PT"][0], d["b_PT"][0]
            b = nextbank(key); pb = self.bank(b)
            for ch in range(2):
                for hh in range(2):
                    u = ch * 2 + hh
                    self.MM(pb[:, u * 64:(u + 1) * 64], AakT[:, u, :], tok[:, ch * 4 + 0, hs(hh)], [d["b_A"], d["b_tok"]], [self.psb[2 * b]])
            self.CP("act", d["AV"], pb[:, 0:256].rearrange("p (u x) -> p u x", u=4), [self.psb[2 * b]], [d["b_AV"]])
            yield
            b = nextbank(key); pb = self.bank(b)
            pbv = pb.rearrange("p (c w h x) -> p c w h x", c=2, w=2, h=2)
            for ch in range(2):
                for hh in range(2):
                    u = ch * 2 + hh
                    self.MM(pbv[:, ch, 0, hh, :], TinvT[:, u, :], tok[:, ch * 4 + 1, hs(hh)], [bT, d["b_tok"]], self.bankb(b))
                    self.MM(pbv[:, ch, 1, hh, :], TinvT[:, u, :], d["AV"][:, u, :], [bT, d["b_AV"]], self.bankb(b))
            self.ACT(d["WU"].rearrange("p c w h x -> p (c w h x)"), pb, AF.Copy, self.bankb(b), [d["b_WU"]], scale=-1.0)
            yield
            WU = d["WU"]
            b = nextbank(key); pb = self.bank(b)
            for ch in range(2):
                Wn = WU[:, ch, 0].rearrange("p h x -> p (h x)")
                Un = WU[:, ch, 1].rearrange("p h x -> p (h x)")
                self.MM(pb[:, ch * 128:(ch + 1) * 128], Wn, tok[:, ch * 4 + 2, :], [d["b_WU"], d["b_tok"]], [self.psb[2 * b]])
                self.MM(pb[:, 256 + ch * 128:256 + (ch + 1) * 128], tok[:, ch * 4 + 3, :], tok[:, ch * 4 + 0, :], [d["b_tok"]], [self.psb[2 * b + 1]], start=True, stop=False)
                self.MM(pb[:, 256 + ch * 128:256 + (ch + 1) * 128], tok[:, ch * 4 + 2, :], Un, [d["b_tok"], d["b_WU"]], [self.psb[2 * b + 1]], start=False, stop=True)
            self.TT("dve", d["McT"], pb[:, 0:256].rearrange("p (c x) -> p c x", c=2), self.blkf.unsqueeze(1).to_broadcast([128, 2, 128]), ALU.mult, [self.psb[2 * b], self.bc], [d["b_MN"]])
            for ch in range(2):
                for hh in range(2):
                    self.CP("dve" if hh == 0 else "act", d["Nc"][hs(hh), ch, :], pb[hs(hh), 256 + ch * 128 + hh * 64:256 + ch * 128 + (hh + 1) * 64], [self.psb[2 * b + 1]], [d["b_Nc"]])
            b = nextbank(key); pq = self.bank(b)
            for ch in range(2):
                for hh in range(2):
                    u = ch * 2 + hh
                    self.MM(pq[hs(hh), ch * 128:(ch + 1) * 128], WU[:, ch, 0, hh, :], ArbT[:, u, :], [d["b_WU"], d["b_A"]], [self.psb[2 * b]])
            self.TT("dve", d["QcT"], pq[:, 0:256].rearrange("p (c x) -> p c x", c=2), d["RgF"].rearrange("p (c x) -> p c x", c=2), ALU.add, [self.psb[2 * b], bin_], [d["b_Q"]])
            yield
            b = nextbank(key); py = self.bank(b)
            for ch in range(2):
                self.MM(py[:, ch * 128:(ch + 1) * 128], Sbd[:, cc, :], d["QcT"][:, ch, :], [b_S[cc], d["b_Q"]], [self.psb[2 * b]], start=True, stop=False)
                for hh in range(2):
                    u = ch * 2 + hh
                    o = py[hs(hh), ch * 128:(ch + 1) * 128]
                    self.MM(o, tok[:, ch * 4 + 0, hs(hh)], ArkT[:, u, :], [d["b_tok"], d["b_A"]], [self.psb[2 * b]], start=False, stop=False)
                    self.MM(o, WU[:, ch, 1, hh, :], ArbT[:, u, :], [d["b_WU"], d["b_A"]], [self.psb[2 * b]], start=False, stop=(hh == 1))
                pst = py[:, 256 + ch * 64:256 + (ch + 1) * 64]
                self.MM(pst, d["McT"][:, ch, :], Sb[:, cc, :], [d["b_MN"], b_S[cc]], [self.psb[2 * b + 1]])
                self.STT("dve", Sf[:, cc, :], Sf[:, cc, :], d["gC"][:, ch:ch + 1], pst, ALU.mult, ALU.add, [self.psb[2 * b + 1], bin_, b_S[cc]], [b_S[cc]])
                self.TT("dve", Sf[:, cc, :], Sf[:, cc, :], d["Nc"][:, ch, :], ALU.add, [d["b_Nc"], b_S[cc]], [b_S[cc]])
                self.CP("dve", Sb[:, cc, :], Sf[:, cc, :], [b_S[cc]], [b_S[cc]])
                for hh in range(2):
                    self.CP("pool", Sbd[hs(hh), cc, hh * 64:(hh + 1) * 64], Sf[hs(hh), cc, :], [b_S[cc]], [b_S[cc]])
                yield
            y, yb, yc, rs = d["y"], d["yb"], d["yc"], d["rs"]
            by = d["b_y"]
            self.CP("dve", y, py[:, 0:256], [self.psb[2 * b]], [by])
            byb = d["b_X"][1]
            self.CP("act", yb, py[:, 0:256], [self.psb[2 * b]], [byb])
            b2 = nextbank(key); pm_ = self.bank(b2)
            self.MM(pm_[:, 0:256], self.onesblk, yb, [self.bc, byb], [self.psb[2 * b2]])
            self.STT("dve", yc, pm_[:, 0:256], -1.0 / 64, y, ALU.mult, ALU.add, [self.psb[2 * b2], by], [by])
            self.ACT(yb, yc, AF.Square, [by], [byb])
            self.MM(pm_[:, 256:512], self.onesblk, yb, [self.bc, byb], [self.psb[2 * b2 + 1]])
            self.ACT(rs, pm_[:, 256:512], AF.Sqrt, [self.psb[2 * b2 + 1], self.bc], [by], bias=self.eps(1), scale=1.0 / 64)
            self.RECIP(rs, rs, [by], [by])
            self.TT("dve", yc, yc, rs, ALU.mult, [by], [by])
            self.TS("dve", yc, yc, col_lnw(cc), col_lnb(cc), ALU.mult, ALU.add, [by, self.bc], [by])
            self.TT("pool", yc, yc, d["bonus"], ALU.add, [by, bin_], [by])
            self.TT("dve", mixT[s][:, cc, :], yc, d["gate"], ALU.mult, [by, bin_], [b_mix[s][cc]])
            yield

        col_lnw = lambda cc: pc[:, 50 + cc:51 + cc]
        col_lnb = lambda cc: pc[:, 56 + cc:57 + cc]

        def unit(bi, cc, sl):
            s = bi % 2
            yield ("wait", f"pmready{bi}")
            yield ("acq", f"slot{sl}")
            yield ("acq", "ft")
            if cc == 0:
                yield ("acq", f"mix{s}")
            yield from prep(bi, cc, sl)
            if self.alg < 1:
                yield ("rel", "ft")
                if cc == 5:
                    yield ("set", f"pmfree{bi + 1}")
                yield ("rel", f"slot{sl}")
                yield ("set", f"udone{bi}_{cc}")
                return
            yield ("rel", "ft")
            if cc == 5:
                yield ("set", f"pmfree{bi + 1}")
            yield from algo(bi, cc, sl)
            yield ("rel", f"slot{sl}")
            yield ("set", f"udone{bi}_{cc}")

        def attn(bi):
            s = bi % 2
            yield ("wait", f"pmready{bi}")
            for tt in range(2):
                if "attn" in self.skip:
                    continue
                self.attention_tile(qT[s], b_qT[s], slice(tt * 128, (tt + 1) * 128), kT, Vm, b_kv,
                                    mixT[s][:, 6:8, tt * 128:(tt + 1) * 128], b_mix[s][6 + tt % 2], (0, 1), AS)
                yield
            for cc in range(6):
                yield ("wait", f"udone{bi}_{cc}")
            t0 = bi * NB
            if "store" not in self.skip:
              self.P.dma("sp", lmix[s], lambda h: h.dma_start(out=self.mixT_d.rearrange("(c p) t -> p c t", p=128)[:, :, t0:t0 + NB], in_=mixT[s]),
                       [bb for bb in b_mix[s]], [self.b_mixscr[bi]])
            yield ("rel", f"mix{s}")

        def frontw(bi):
            yield from front(bi)
            yield ("set", f"pmready{bi}")

        self.b_mixscr = [P.buf(f"mixscr{i}") for i in range(NBLK)]
        gens = [frontw(0)]
        u = 0
        pre = ["pmfree0"]
        for bi in range(self.nblk):
            for cc in range(6):
                if cc < self.ncc:
                    gens.append(unit(bi, cc, u % NSLOT))
                    u += 1
                else:
                    pre.append(f"udone{bi}_{cc}")
                    if cc == 5:
                        pre.append(f"pmfree{bi + 1}")
            if bi + 1 < self.nblk:
                gens.append(frontw(bi + 1))
            gens.append(attn(bi))
        drive(gens, 6, preset=pre)
        if "pm" in self.dbg:
            self.dump("pm", pm, [128, 20, NB], b_pm)
        self.dump("mixT", mixT[(self.nblk - 1) % 2], [128, 8, NB], [bb for bb in b_mix[(self.nblk - 1) % 2]], BF16)
        self.l0_fence = P.fence()
        A.release()

    def resid_from_scratch(self, li):
        A, P = self.A, self.P
        fence = P.fence()
        nb = lambda n=None: P.buf(n, after=fence)
        self.X = A.alloc([NT, D], F32)
        self.b_X = [nb(f"X{i}") for i in range(NT)]
        A.mark()
        wout = A.alloc([8, D], BF16); b_w = nb()
        l = P.lane("wout0")
        self.LOAD("pool", l, wout, self.wout_d[li].rearrange("(c p) n -> p c n", p=128), [b_w])
        mt = [A.alloc([8, 128], BF16) for _ in range(2)]; b_mt = [nb(), nb()]
        lm = [P.lane("mt0"), P.lane("mt1")]
        lxx = [P.lane("xx0"), P.lane("xx1")]
        scr = self.mixT_d.rearrange("(c p) t -> p c t", p=128)
        for tt in range(NT):
            k = tt % 2
            self.LOAD("sp", lm[k], mt[k], scr[:, :, tt * 128:(tt + 1) * 128], [b_mt[k]], R=[self.b_mixscr[tt // 2]])
            self.LOAD("sp", lxx[k], self.X[:, tt, :], self.x_d[tt * 128:(tt + 1) * 128, :], [self.b_X[tt]])
            for half in range(2):
                b = (tt * 2 + half) % 8
                pb = self.bank(b)
                for c in range(8):
                    self.MM(pb, mt[k][:, c, :], wout[:, c, half * 512:(half + 1) * 512], [b_mt[k], b_w], self.bankb(b), start=(c == 0), stop=(c == 7))
                self.TT("dve", self.X[:, tt, half * 512:(half + 1) * 512], pb, self.X[:, tt, half * 512:(half + 1) * 512], ALU.add, self.bankb(b) + [self.b_X[tt]], [self.b_X[tt]])
        self.dump("X0a", self.X, [128, NT, D], self.b_X)
        A.release()

    def final_out(self):
        A, P = self.A, self.P
        fence = P.fence()
        nb = lambda n=None: P.buf(n, after=fence)
        A.mark()
        gb = A.alloc([D], F32); b_gb = nb()
        l = P.lane("gfin")
        self.LOAD("sp", l, gb, self.grow_d[5, :].partition_broadcast(128), [b_gb])
        ot = [A.alloc([D], F32) for _ in range(2)]; b_ot = [nb(), nb()]
        sq = A.alloc([D], BF16); stt = [A.alloc([4], F32) for _ in range(2)]; b_t = [nb(), nb()]
        lo = [P.lane("out0"), P.lane("out1")]
        self.out_lanes += lo
        for tt in range(NT):
            k = tt % 2
            xs = self.X[:, tt, :]
            self.ACT(sq, xs, AF.Square, [self.b_X[tt]], [b_t[k]], accum=stt[k][:, 0:1])
            self.ACT(stt[k][:, 1:2], stt[k][:, 0:1], AF.Sqrt, [b_t[k], self.bc], [b_t[k]], bias=self.eps(0), scale=1.0 / D)
            self.RECIP(stt[k][:, 2:3], stt[k][:, 1:2], [b_t[k]], [b_t[k]])
            self.STT("dve", ot[k], xs, stt[k][:, 2:3], gb, ALU.mult, ALU.mult, [self.b_X[tt], b_gb, b_t[k]], [b_ot[k]])
            self.P.dma("sp", lo[k], lambda h, k=k, tt=tt: h.dma_start(out=self.y_d[tt * 128:(tt + 1) * 128, :], in_=ot[k]), [b_ot[k]], ())
        A.release()

    def ffn_phase(self, li):
        A, P = self.A, self.P
        fence = P.fence()
        nb = lambda n=None: P.buf(n, after=fence)
        A.mark()
        moe = (li == 1)
        nT2 = A.alloc([8, T], BF16); b_nT2 = [nb(f"nT2_{i}") for i in range(NT)]
        gb = A.alloc([D], F32); b_gb = nb()
        lg = P.lane(f"g2_{li}")
        self.LOAD("sp", lg, gb, self.grow_d[2 if li == 0 else 4, :].partition_broadcast(128), [b_gb])
        if moe:
            comb = A.alloc([NT, 8], F32); b_comb = [nb() for _ in range(NT)]
        A.mark()
        xnf = [A.alloc([D], F32) for _ in range(2)] if moe else None
        xn = [A.alloc([D], BF16) for _ in range(2)]; b_xn = [nb(), nb()]
        sq = A.alloc([D], BF16); stt = [A.alloc([4], F32) for _ in range(2)]; b_t = [nb(), nb()]
        if moe:
            wr = A.alloc([8, 8], F32); b_wr = nb()
            lr = P.lane("router")
            self.LOAD("sp", lr, wr, self.rt_d.rearrange("(c p) e -> p c e", p=128), [b_wr])
            nTf = [A.alloc([8, 128], F32) for _ in range(2)]; b_nTf = [nb(), nb()]
            rs_ = [A.alloc([64], F32) for _ in range(2)]; b_rs = [nb(), nb()]
        for tt in range(NT):
            k = tt % 2
            xs = self.X[:, tt, :]
            self.ACT(sq, xs, AF.Square, [self.b_X[tt]], [b_t[k]], accum=stt[k][:, 0:1])
            self.ACT(stt[k][:, 1:2], stt[k][:, 0:1], AF.Sqrt, [b_t[k], self.bc], [b_t[k]], bias=self.eps(0), scale=1.0 / D)
            self.RECIP(stt[k][:, 2:3], stt[k][:, 1:2], [b_t[k]], [b_t[k]])
            if moe:
                self.STT("dve", xnf[k], xs, stt[k][:, 2:3], gb, ALU.mult, ALU.mult, [self.b_X[tt], b_gb, b_t[k]], [b_xn[k]])
                self.CP("act", xn[k], xnf[k], [b_xn[k]], [b_xn[k]])
            else:
                self.STT("dve", xn[k], xs, stt[k][:, 2:3], gb, ALU.mult, ALU.mult, [self.b_X[tt], b_gb, b_t[k]], [b_xn[k]])
            b = tt % 2
            pb = self.bank(b).bitcast(BF16)
            for c in range(8):
                self.TR(pb[:, c * 128:(c + 1) * 128], xn[k][:, c * 128:(c + 1) * 128], self.identb, [b_xn[k], self.bc], self.bankb(b))
            self.CP("act" if tt % 2 == 0 else "dve", nT2[:, :, tt * 128:(tt + 1) * 128], pb.rearrange("p (c t) -> p c t", c=8), self.bankb(b), [b_nT2[tt]])
            if moe:
                pf = self.ps[:, 1024 + k * 1024:2048 + k * 1024]
                pfb = self.bankb(2 + 2 * k) + self.bankb(3 + 2 * k)
                for c in range(8):
                    self.TR(pf[:, c * 128:(c + 1) * 128], xnf[k][:, c * 128:(c + 1) * 128], self.identf, [b_xn[k], self.bc], pfb)
                self.CP("act", nTf[k], pf.rearrange("p (c t) -> p c t", c=8), pfb, [b_nTf[k]])
                pl = self.bank(6 + k)
                for c in range(8):
                    self.MM(pl[:, 0:8], nTf[k][:, c, :], wr[:, c, :], [b_nTf[k], b_wr], self.bankb(6 + k), start=(c == 0), stop=(c == 7))
                r = rs_[k]; br = b_rs[k]
                lgt, m1, eq1, l2, m2, eq2, dl, ee, g1, g2 = (r[:, 0:8], r[:, 8:9], r[:, 16:24], r[:, 24:32], r[:, 9:10], r[:, 32:40], r[:, 10:11], r[:, 11:12], r[:, 12:13], r[:, 13:14])
                self.CP("dve", lgt, pl[:, 0:8], self.bankb(6 + k), [br])
                P.op("dve", lambda h, m1=m1, lgt=lgt: h.tensor_reduce(m1, lgt, AX.X, ALU.max), [br], [br])
                self.TS("dve", eq1, lgt, m1, None, ALU.is_equal, None, [br], [br])
                self.STT("dve", l2, eq1, -1e30, lgt, ALU.mult, ALU.add, [br], [br])
                P.op("dve", lambda h, m2=m2, l2=l2: h.tensor_reduce(m2, l2, AX.X, ALU.max), [br], [br])
                self.TS("dve", eq2, l2, m2, None, ALU.is_equal, None, [br], [br])
                self.TT("dve", dl, m2, m1, ALU.subtract, [br], [br])
                self.ACT(ee, dl, AF.Exp, [br], [br])
                self.TS("dve", g1, ee, 1.0, None, ALU.add, None, [br], [br])
                self.RECIP(g1, g1, [br], [br])
                self.TT("dve", g2, ee, g1, ALU.mult, [br], [br])
                self.TS("dve", eq1, eq1, g1, None, ALU.mult, None, [br], [br])
                self.STT("dve", comb[:, tt, :], eq2, g2, eq1, ALU.mult, ALU.add, [br], [b_comb[tt]])
        if moe:
            self.dump("comb", comb, [128, NT, 8], b_comb)
        A.release()
        fence = P.fence()
        GROUPS = [(0, 8), (8, 8), (16, 6)]
        NWG, NWD = 2, 2
        wgu = [A.alloc([8, 2, 256], BF16) for _ in range(NWG)]; b_wgu = [nb() for _ in range(NWG)]
        l_wgu = [P.lane(f"wgu{li}_{i}") for i in range(NWG)]
        wd = [A.alloc([8, D], BF16) for _ in range(NWD)]; b_wd = [nb() for _ in range(NWD)]
        l_wd = [P.lane(f"wd{li}_{i}") for i in range(NWD)]
        hT = A.alloc([8, T], BF16); b_h = [[nb() for _ in range(4)] for _ in range(8)]
        sg = [A.alloc([512], F32) for _ in range(2)]; b_sg = [nb(), nb()]
        nexp = 8 if moe else 1
        if "ffn_all" in self.skip:
            nexp = 0
        si = 0
        gi = 0
        pgu = 0
        pdn = 0
        for e in range(nexp):
            gu_src = (self.mgu_d[e] if moe else self.fgu_d).rearrange("(c p) n -> p c n", p=128)
            dn_src = self.mdn_d[e] if moe else self.fdn_d
            for (g0, G) in GROUPS:
                ws = gi % NWD
                self.LOAD("pool", l_wd[ws], wd[ws][:, 0:G, :], dn_src[g0 * 128:(g0 + G) * 128, :].rearrange("(j p) n -> p j n", p=128), [b_wd[ws]])
                for sl_ in range(G // 2):
                    k = si % NWG
                    hc0 = g0 + 2 * sl_
                    self.LOAD("pool", l_wgu[k], wgu[k][:, :, 0, :], gu_src[:, :, hc0 * 128:hc0 * 128 + 256], [b_wgu[k]])
                    self.LOAD("pool", l_wgu[k], wgu[k][:, :, 1, :], gu_src[:, :, DFF + hc0 * 128:DFF + hc0 * 128 + 256], [b_wgu[k]])
                    for j in range(2):
                        hl = 2 * sl_ + j
                        for tb in range(4):
                            bg = (pgu % 2) * 2
                            pgu += 1
                            pg, pu = self.bank(bg), self.bank(bg + 1)
                            rd = [b_wgu[k]] + b_nT2[tb * 4:(tb + 1) * 4]
                            for c in range(8):
                                self.MM(pg, wgu[k][:, c, 0, j * 128:(j + 1) * 128], nT2[:, c, tb * 512:(tb + 1) * 512], rd, self.bankb(bg), start=(c == 0), stop=(c == 7))
                            for c in range(8):
                                self.MM(pu, wgu[k][:, c, 1, j * 128:(j + 1) * 128], nT2[:, c, tb * 512:(tb + 1) * 512], rd, self.bankb(bg + 1), start=(c == 0), stop=(c == 7))
                            q = pgu % 2
                            self.ACT(sg[q], pg, AF.Silu, self.bankb(bg), [b_sg[q]])
                            self.TT("dve", hT[:, hl, tb * 512:(tb + 1) * 512], sg[q], pu, ALU.mult, [b_sg[q]] + self.bankb(bg + 1), [b_h[hl][tb]])
                    si += 1
                for tt in range((1 if "ffn_dn1" in self.skip else NT) if "ffn_dn" not in self.skip else 0):
                    for half in range(2):
                        bd = 4 + pdn % 4
                        pdn += 1
                        pd = self.bank(bd)
                        for hl in range(G):
                            self.MM(pd, hT[:, hl, tt * 128:(tt + 1) * 128], wd[ws][:, hl, half * 512:(half + 1) * 512], [b_h[hl][tt // 4], b_wd[ws]], self.bankb(bd), start=(hl == 0), stop=(hl == G - 1))
                        xs = self.X[:, tt, half * 512:(half + 1) * 512]
                        if "ffn_noacc" in self.skip:
                            continue
                        if moe:
                            self.STT("dve", xs, pd, comb[:, tt, e:e + 1], xs, ALU.mult, ALU.add, self.bankb(bd) + [b_comb[tt], self.b_X[tt]], [self.b_X[tt]])
                        else:
                            self.TT("dve", xs, pd, xs, ALU.add, self.bankb(bd) + [self.b_X[tt]], [self.b_X[tt]])
                gi += 1
        self.dump("X0b" if li == 0 else "X1b", self.X, [128, NT, D], self.b_X)
        A.release()

    def layer1_mixer(self):
        A, P = self.A, self.P
        fence = P.fence()
        nb = lambda n=None: P.buf(n, after=fence)
        A.mark()
        kT = A.alloc([4, 256], BF16); Vm = A.alloc([2, 256], BF16); b_kv = nb("kv1")
        A.mark()
        wkv = A.alloc([8, 512], BF16); b_wkv = nb("wkv1")
        self.kv_prep(1, kT, Vm, b_kv, wkv, b_wkv, P.lane("wkv1"))
        A.release()
        fence = P.fence()
        win = A.alloc([8, 1792], BF16); b_win = nb()
        wout = A.alloc([8, D], BF16); b_wout = nb()
        gb = A.alloc([D], F32); lnG = A.alloc([MIX], F32); lnB = A.alloc([MIX], F32); b_gb = nb()
        wsT = A.alloc([12, 128], BF16); wsF = A.alloc([12, 128], F32); b_ws = nb()
        bfm = A.alloc([6, 128], F32); b_bfm = nb()
        ll = [P.lane(f"l1w{i}") for i in range(5)]
        self.LOAD("sp", ll[0], gb, self.grow_d[3, :].partition_broadcast(128), [b_gb])
        self.LOAD("sp", ll[0], lnG, self.lnr_d[0, :].partition_broadcast(128), [b_gb])
        self.LOAD("sp", ll[0], lnB, self.lnr_d[1, :].partition_broadcast(128), [b_gb])
        self.LOAD("pool", ll[1], win, self.win1_d.rearrange("(c p) n -> p c n", p=128), [b_win])
        self.LOAD("pool", ll[2], wout, self.wout_d[1].rearrange("(c p) n -> p c n", p=128), [b_wout])
        self.LOAD("sp", ll[3], wsF, self.wsT_d[:, :, :], [b_ws])
        self.TT("dve", wsT, wsF, self.mAT[:, 128:256].unsqueeze(1).to_broadcast([128, 12, 128]), ALU.mult, [b_ws, self.bc], [b_ws])
        bsv = self.bs_d.rearrange("(c h) t -> h c t", h=2)
        for hh in range(2):
            self.LOAD("sp", ll[4], bfm[hh * 64:(hh + 1) * 64, :, :], bsv[hh].partition_broadcast(64), [b_bfm])
        xn = [A.alloc([D], BF16) for _ in range(2)]; b_xn = [nb(), nb()]
        sq = A.alloc([D], BF16); stt = [A.alloc([8], F32) for _ in range(2)]; b_t = [nb(), nb()]
        nTt = [A.alloc([8, 128], BF16) for _ in range(2)]; b_nTt = [nb(), nb()]
        uT = [A.alloc([6, 128], F32) for _ in range(2)]; b_uT = [nb(), nb()]
        qTt = [A.alloc([2, 128], BF16) for _ in range(2)]; b_qTt = [nb(), nb()]
        vg = [A.alloc([MIX], F32) for _ in range(2)]; b_vg = [nb(), nb()]
        vtok = [A.alloc([MIX], BF16) for _ in range(2)]; b_vtok = [nb(), nb()]
        mixf = [A.alloc([6, 128], F32) for _ in range(2)]; b_mixf = [nb(), nb()]
        mixTt = [A.alloc([8, 128], BF16) for _ in range(2)]; b_mixy = [nb(), nb()]; b_mixo = [nb(), nb()]
        AS = self.attn_scratch()
        def stageA(tt):
            k = tt % 2
            B0 = 4 * k
            xs = self.X[:, tt, :]
            self.rms_tile(xs, self.b_X[tt], gb, b_gb, xn[k], b_xn[k], sq, stt[k], b_t[k])
            pb = self.bank(B0 + 2).bitcast(BF16)
            for c in range(8):
                self.TR(pb[:, c * 128:(c + 1) * 128], xn[k][:, c * 128:(c + 1) * 128], self.identb, [b_xn[k], self.bc], self.bankb(B0 + 2))
            self.CP("act", nTt[k], pb.rearrange("p (c t) -> p c t", c=8), self.bankb(B0 + 2), [b_nTt[k]])
            p1, p2 = self.bank(B0), self.bank(B0 + 1)
            for fc in range(6):
                dst = p1[:, fc * 128:(fc + 1) * 128] if fc < 4 else p2[:, (fc - 4) * 128:(fc - 3) * 128]
                bb = self.bankb(B0) if fc < 4 else [self.psb[2 * B0 + 2]]
                for c in range(8):
                    self.MM(dst, win[:, c, fc * 128:(fc + 1) * 128], nTt[k][:, c, :], [b_win, b_nTt[k]], bb, start=(c == 0), stop=(c == 7))
            for j in range(2):
                for c in range(8):
                    self.MM(p2[:, 256 + j * 128:256 + (j + 1) * 128], win[:, c, 1536 + j * 128:1536 + (j + 1) * 128], nTt[k][:, c, :], [b_win, b_nTt[k]], [self.psb[2 * B0 + 3]], start=(c == 0), stop=(c == 7))
            uflat = uT[k].rearrange("p c t -> p (c t)")
            self.ACT(uflat, self.ps[:, B0 * 512:B0 * 512 + 768], AF.Gelu, self.bankb(B0) + [self.psb[2 * B0 + 2]], [b_uT[k]])
            self.CP("dve", qTt[k], p2[:, 256:512].rearrange("p (c t) -> p c t", c=2), [self.psb[2 * B0 + 3]], [b_qTt[k]])
            p3, p4 = self.bank(B0 + 3), self.bank(B0 + 2)
            for c in range(8):
                self.MM(p3, nTt[k][:, c, :], win[:, c, 768:1280], [b_win, b_nTt[k]], self.bankb(B0 + 3), start=(c == 0), stop=(c == 7))
            for c in range(8):
                self.MM(p4[:, 0:256], nTt[k][:, c, :], win[:, c, 1280:1536], [b_win, b_nTt[k]], [self.psb[2 * B0 + 4]], start=(c == 0), stop=(c == 7))
            st_ = stt[k]
            self.ACT(vg[k][:, 0:512], p3, AF.Gelu, self.bankb(B0 + 3), [b_vg[k]], accum=st_[:, 3:4])
            self.ACT(vg[k][:, 512:768], p4[:, 0:256], AF.Gelu, [self.psb[2 * B0 + 4]], [b_vg[k]], accum=st_[:, 4:5])
            self.TT("dve", st_[:, 5:6], st_[:, 3:4], st_[:, 4:5], ALU.add, [b_vg[k]], [b_vg[k]])
            self.TS("dve", st_[:, 5:6], st_[:, 5:6], 1.0 / MIX, None, ALU.mult, None, [b_vg[k]], [b_vg[k]])
            self.TS("dve", vg[k], vg[k], st_[:, 5:6], None, ALU.subtract, None, [b_vg[k]], [b_vg[k]])
            self.ACT(sq[:, 0:MIX], vg[k], AF.Square, [b_vg[k]], [b_vg[k]], accum=st_[:, 6:7])
            self.ACT(st_[:, 7:8], st_[:, 6:7], AF.Sqrt, [b_vg[k], self.bc], [b_vg[k]], bias=self.eps(2), scale=1.0 / MIX)
            self.RECIP(st_[:, 7:8], st_[:, 7:8], [b_vg[k]], [b_vg[k]])
            self.STT("dve", vg[k], vg[k], st_[:, 7:8], lnG, ALU.mult, ALU.mult, [b_vg[k], b_gb], [b_vg[k]])
            self.TT("dve", vtok[k], vg[k], lnB, ALU.add, [b_vg[k], b_gb], [b_vtok[k]])

        def stageB(tt):
            k = tt % 2
            B0 = 4 * k
            p5, p6 = self.bank(B0), self.bank(B0 + 1)
            for g in range(12):
                cc, hp = g // 2, (g % 2) * 64
                dst = p5[hp:hp + 64, cc * 128:(cc + 1) * 128] if cc < 4 else p6[hp:hp + 64, (cc - 4) * 128:(cc - 3) * 128]
                bb = self.bankb(B0) if cc < 4 else [self.psb[2 * B0 + 2]]
                self.MM(dst, vtok[k][:, g * 64:(g + 1) * 64], wsT[:, g, :], [b_vtok[k], b_ws], bb)
            self.TT("dve", mixf[k][:, 0:4, :], p5.rearrange("p (c t) -> p c t", c=4), bfm[:, 0:4, :], ALU.add, self.bankb(B0) + [b_bfm], [b_mixf[k]])
            self.TT("dve", mixf[k][:, 4:6, :], p6[:, 0:256].rearrange("p (c t) -> p c t", c=2), bfm[:, 4:6, :], ALU.add, [self.psb[2 * B0 + 2], b_bfm], [b_mixf[k]])
            self.TT("pool", mixTt[k][:, 0:6, :], mixf[k], uT[k], ALU.mult, [b_mixf[k], b_uT[k]], [b_mixy[k]])
            self.attention_tile(qTt[k], b_qTt[k], slice(0, 128), kT, Vm, b_kv, mixTt[k][:, 6:8, :], b_mixo[k], (B0 + 2, B0 + 3), AS)
            for half in range(2):
                b = B0 + half
                pw = self.bank(b)
                for c in range(8):
                    self.MM(pw, mixTt[k][:, c, :], wout[:, c, half * 512:(half + 1) * 512], [b_mixy[k], b_mixo[k], b_wout], self.bankb(b), start=(c == 0), stop=(c == 7))
                xh = self.X[:, tt, half * 512:(half + 1) * 512]
                self.TT("dve", xh, pw, xh, ALU.add, self.bankb(b) + [self.b_X[tt]], [self.b_X[tt]])
        for i in range(NT + 1):
            if i < NT:
                stageA(i)
            if i >= 1:
                stageB(i - 1)
        self.dump("X1a", self.X, [128, NT, D], self.b_X)
        A.release()

_CACHE = {}


def _prep_inputs(inp):
    f = lambda a: np.ascontiguousarray(a, dtype=np.float32)
    pcols = np.zeros((128, 64), np.float32)
    pcols[:, 0:20] = inp["rwkv_mu"][0].reshape(20, 128).T
    for i, k in enumerate(["rwkv_w0", "rwkv_a0", "rwkv_k_k", "rwkv_k_a", "rwkv_r_k", "rwkv_lnx_w", "rwkv_lnx_b"]):
        pcols[:, 20 + 6 * i:26 + 6 * i] = inp[k][0].reshape(6, 128).T
    grows = np.stack([inp["mem_norm_g"], inp["norm1_g"][0], inp["norm2_g"][0], inp["norm1_g"][1], inp["norm2_g"][1], inp["final_norm_g"]]).astype(np.float32)
    shared = {
        "grows": f(grows), "pcols": pcols,
        "rwkv_w_in": f(inp["rwkv_w_in"][0]), "rwkv_w2": f(inp["rwkv_w2"][0]), "rwkv_a2": f(inp["rwkv_a2"][0]), "rwkv_g2": f(inp["rwkv_g2"][0]),
        "w_kv_mem": f(inp["w_kv_mem"]), "w_out": f(inp["w_out"]),
        "ffn_w_gu": f(inp["ffn_w_gu"][0]), "ffn_w_down": f(inp["ffn_w_down"][0]),
        "gmlp_w_in": f(inp["gmlp_w_in"][0]),
        "gmlp_ln": f(np.stack([inp["gmlp_v_ln_g"][0], inp["gmlp_v_ln_b"][0]])),
        "gmlp_wsT": f(np.transpose(inp["gmlp_w_s"][0], (2, 0, 1))),
        "gmlp_bs": f(inp["gmlp_b_s"][0]),
        "moe_router": f(inp["moe_router"][0]), "moe_w_gu": f(inp["moe_w_gu"][0]), "moe_w_down": f(inp["moe_w_down"][0]),
    }
    return shared


def run(inp, cores=8, dbg=(), stage=99):
    key = (tuple(dbg), stage)
    if key not in _CACHE:
        k = K(dbg=dbg, stage=stage)
        k.build()
        _CACHE[key] = k
    k = _CACHE[key]
    shared = _prep_inputs(inp)
    in_maps = []
    for b in range(cores):
        m = dict(shared)
        m["x"] = np.ascontiguousarray(inp["x"][b], dtype=np.float32)
        m["mem"] = np.ascontiguousarray(inp["mem"][b], dtype=np.float32)
        in_maps.append(m)
    res = run_bass_kernel_spmd(k.nc, in_maps, core_ids=list(range(cores)))
    return res, k


def kernel(**inputs):
    res, k = run(inputs)
    out = np.stack([np.asarray(r["y"], dtype=np.float32) for r in res.results], axis=0)
    return out
```

```python
import contextlib
import math
import numpy as np
import concourse.bass as bass
import concourse.mybir as mybir
from concourse.bass_utils import run_bass_kernel_spmd

F32 = mybir.dt.float32
BF16 = mybir.dt.bfloat16
AF = mybir.ActivationFunctionType
ALU = mybir.AluOpType
AX = mybir.AxisListType

T = 2048
D = 1024
NT = 16
NB = 256
NBLK = T // NB
MIX = 768
DFF = 2816
NHC = 22
C0 = math.exp(-0.5)
RMS_EPS = 1e-6
GN_EPS = 64e-5
LN_EPS = 1e-5


class Buf:
    __slots__ = ("name", "w", "r")

    def __init__(self, name):
        self.name = name
        self.w = None
        self.r = {}


class Lane:
    __slots__ = ("name", "sem")

    def __init__(self, name):
        self.name = name
        self.sem = None


class Op:
    __slots__ = ("idx", "eng", "fn", "lane", "deps", "signal", "sig", "order", "cost", "seg", "fin", "done", "pin")

    def __init__(self, idx, eng, fn, lane):
        self.idx = idx
        self.eng = eng
        self.fn = fn
        self.lane = lane
        self.deps = set()
        self.signal = False
        self.sig = None
        self.order = set()
        self.cost = 300.0
        self.seg = 0
        self.fin = 0.0
        self.done = False
        self.pin = False


ENGS = ("pe", "act", "dve", "pool", "sp")
import os as _os
PINS = set(_os.environ.get("PINS", "recip,scan,reduce,psum").split(","))


class Prog:
    def __init__(self, nc):
        self.nc = nc
        self.ops = []
        self.lanes = []
        self.nbuf = 0
        self.last = {}
        self.seg = 0
        self.cur_cost = None

    def buf(self, name=None, after=None):
        self.nbuf += 1
        b = Buf(name or f"b{self.nbuf}")
        if after:
            b.r = {(d.lane if d.lane is not None else d.eng): d for d in after}
        return b

    def lane(self, name=None):
        l = Lane(name or f"lane{len(self.lanes)}")
        self.lanes.append(l)
        return l

    def fence(self):
        self.seg += 1
        return list(self.last.values())

    def op(self, eng, fn, reads=(), writes=(), lane=None, cost=None):
        o = Op(len(self.ops), eng, fn, lane)
        o.seg = self.seg
        if cost is not None:
            o.cost = cost
        deps = set()
        order = set()
        key = lane if lane is not None else eng
        for b in reads:
            d = b.w
            if d is not None:
                if d.lane is None and lane is None and d.eng == eng and eng == "pe":
                    order.add(d)
                else:
                    deps.add(d)
            prev = b.r.get(key)
            if prev is not None:
                order.add(prev)
        for b in writes:
            for d in ([b.w] if b.w is not None else []) + list(b.r.values()):
                if d.lane is None and lane is None and d.eng == eng:
                    order.add(d)
                    continue
                if lane is not None and d.lane is lane:
                    order.add(d)
                    continue
                deps.add(d)
        deps.discard(o)
        order.discard(o)
        o.deps = deps
        o.order = order
        for d in deps:
            d.signal = True
        if lane is not None:
            o.signal = True
        for b in reads:
            b.r[lane if lane is not None else eng] = o
        for b in writes:
            b.w = o
            b.r = {}
        self.ops.append(o)
        self.last[lane if lane is not None else eng] = o
        return o

    def pe(self, fn, reads=(), writes=()):
        return self.op("pe", fn, reads, writes)

    def dma(self, q, lane, fn, reads=(), writes=()):
        return self.op(q, fn, reads, writes, lane=lane)

    def schedule(self, window=None, hop=200.0):
        import os
        if window is None:
            window = int(os.environ.get("SWIN", "40"))
        seng = os.environ.get("SENG")
        seng = set(seng.split(",")) if seng else None
        per = {e: [] for e in ENGS}
        if not os.environ.get("SCHED"):
            for o in self.ops:
                per[o.eng].append(o)
            return per
        free = {e: 0.0 for e in ENGS}
        segs = {}
        for o in self.ops:
            segs.setdefault(o.seg, []).append(o)
        for sg in sorted(segs):
            ops = segs[sg]
            pend = {e: [o for o in ops if o.eng == e] for e in ENGS}
            t0 = max(free.values())
            for e in ENGS:
                free[e] = t0
            remaining = len(ops)
            while remaining:
                best = None
                for e in ENGS:
                    lst = pend[e]
                    if not lst:
                        continue
                    fe = free[e]
                    n = 0
                    win_e = window if (seng is None or e in seng) else 1
                    for o in lst:
                        if n >= win_e or (o.pin and n > 0):
                            break
                        n += 1
                        ok = True
                        rdy = fe
                        for d in o.order:
                            if not d.done:
                                ok = False
                                break
                        if not ok:
                            continue
                        for d in o.deps:
                            if not d.done:
                                ok = False
                                break
                            t = d.fin + hop
                            if t > rdy:
                                rdy = t
                        if not ok:
                            continue
                        cand = (rdy, o.idx, o, e)
                        if best is None or cand[:2] < best[:2]:
                            best = cand
                        if rdy <= fe:
                            break
                rdy, _, o, e = best
                o.done = True
                if o.lane is not None:
                    o.fin = rdy + 2500.0 + o.cost
                    free[e] = rdy + 150.0
                else:
                    o.fin = rdy + o.cost
                    free[e] = o.fin
                pend[e].remove(o)
                per[e].append(o)
                remaining -= 1
        return per

    def emit(self, final_lanes=()):
        nc = self.nc
        with contextlib.ExitStack() as st:
            sems = {}
            for e in ("pe", "act", "dve", "pool"):
                sems[e] = st.enter_context(nc.semaphore("s_" + e))
            for l in self.lanes:
                l.sem = st.enter_context(nc.semaphore("l_" + l.name))
            cnt = {e: 0 for e in ENGS}
            lcnt = {}
            self.per = self.schedule()
            lastop = {}
            for e in ENGS:
                for o in self.per[e]:
                    lastop[(o.seg, o.lane if o.lane is not None else o.eng)] = o
            for o in self.ops:
                if any(d.seg < o.seg for d in o.deps):
                    nd = set()
                    for d in o.deps:
                        if d.seg < o.seg:
                            d = lastop[(d.seg, d.lane if d.lane is not None else d.eng)]
                            d.signal = True
                        nd.add(d)
                    o.deps = nd
            for o in [o for e in ENGS for o in self.per[e]]:
                if o.lane is not None:
                    c = lcnt.get(o.lane, 0) + 16
                    lcnt[o.lane] = c
                    o.sig = (o.lane.sem, c, 16)
                elif o.signal:
                    cnt[o.eng] += 1
                    o.sig = (sems[o.eng], cnt[o.eng], 1)
            block = st.enter_context(nc.Block())
            per = self.per
            stats = {}

            def run(e, h):
                waited = {}
                nw = 0
                for o in per[e]:
                    need = {}
                    for d in o.deps:
                        s, v, _ = d.sig
                        k = id(s)
                        if v > need.get(k, (None, 0))[1]:
                            need[k] = (s, v)
                    for k, (s, v) in need.items():
                        if waited.get(k, 0) >= v:
                            continue
                        h.wait_ge(s, v)
                        waited[k] = v
                        nw += 1
                    ins = o.fn(h)
                    if o.sig is not None:
                        ins.then_inc(o.sig[0], o.sig[2])
                if e == "sp":
                    for l in final_lanes:
                        if lcnt.get(l, 0) > 0:
                            h.wait_ge(l.sem, lcnt[l])
                stats[e] = (len(per[e]), nw)

            @block.tensor
            def _(h):
                run("pe", h)

            @block.scalar
            def _(h):
                run("act", h)

            @block.vector
            def _(h):
                run("dve", h)

            @block.gpsimd
            def _(h):
                run("pool", h)

            @block.sync
            def _(h):
                run("sp", h)

            self.stats = stats
            self.sigcnt = cnt


def drive(gens, window, preset=()):
    gens = list(gens)
    active = []
    nxt = 0
    locks = {}
    events = set(preset)
    pending = {}
    while nxt < len(gens) or active:
        while len(active) < window and nxt < len(gens):
            active.append(gens[nxt])
            nxt += 1
        progressed = False
        for g in list(active):
            while True:
                req = pending.get(id(g))
                if req is not None:
                    kind, name = req
                    if kind == "acq":
                        if locks.get(name) is None:
                            locks[name] = id(g)
                            pending[id(g)] = None
                        else:
                            break
                    elif kind == "wait":
                        if name in events:
                            pending[id(g)] = None
                        else:
                            break
                try:
                    r = next(g)
                    progressed = True
                except StopIteration:
                    active.remove(g)
                    progressed = True
                    break
                if r is None:
                    break
                kind, name = r
                if kind == "rel":
                    locks[name] = None
                elif kind == "set":
                    events.add(name)
                else:
                    pending[id(g)] = r
        assert progressed, "scheduler deadlock"


class Arena:
    def __init__(self, t, nwords):
        self.t = t
        self.n = nwords
        self.off = 0
        self.marks = []

    def alloc(self, shape, dtype=F32):
        nel = int(np.prod(shape))
        nby = nel * (2 if dtype == BF16 else 4)
        nw = (nby + 3) // 4
        nw = (nw + 1) // 2 * 2
        assert self.off + nw <= self.n, f"SBUF arena overflow: need {self.off + nw} have {self.n}"
        ap = self.t[:, self.off:self.off + nw]
        self.off += nw
        if dtype != F32:
            ap = ap.bitcast(dtype)
        ap = ap[:, 0:nel]
        if len(shape) == 2:
            ap = ap.rearrange("p (a b) -> p a b", a=shape[0])
        elif len(shape) == 3:
            ap = ap.rearrange("p (a b c) -> p a b c", a=shape[0], b=shape[1])
        elif len(shape) == 4:
            ap = ap.rearrange("p (a b c d) -> p a b c d", a=shape[0], b=shape[1], c=shape[2])
        return ap

    def view(self, off, shape, dtype=F32):
        save = self.off
        self.off = off
        ap = self.alloc(shape, dtype)
        self.off = save
        return ap

    def mark(self):
        self.marks.append(self.off)

    def release(self):
        self.off = self.marks.pop()


class K:
    def __init__(self, dbg=(), stage=99, nblk=NBLK, ncc=6, alg=99):
        self.dbg = set(dbg)
        self.stage = stage
        self.nblk = nblk
        self.ncc = ncc
        self.alg = alg
        import os
        self.skip = set(os.environ.get('SKIP', '').split(','))
        self.nc = nc = bass.Bass("TRN2", target_bir_lowering=False)
        self.P = Prog(nc)
        self.dbg_outs = {}
        self.dbg_lanes = []
        din = lambda n, s, d=F32: nc.dram_tensor(n, s, d, kind="ExternalInput").ap()
        self.x_d = din("x", [T, D])
        self.mem_d = din("mem", [256, D])
        self.grow_d = din("grows", [6, D])
        self.pc_d = din("pcols", [128, 64])
        self.win0_d = din("rwkv_w_in", [D, 2816])
        self.w2_d = din("rwkv_w2", [64, MIX])
        self.a2_d = din("rwkv_a2", [64, MIX])
        self.g2_d = din("rwkv_g2", [128, MIX])
        self.wkv_d = din("w_kv_mem", [2, D, 512])
        self.wout_d = din("w_out", [2, D, D])
        self.fgu_d = din("ffn_w_gu", [D, 2 * DFF])
        self.fdn_d = din("ffn_w_down", [DFF, D])
        self.win1_d = din("gmlp_w_in", [D, 1792])
        self.lnr_d = din("gmlp_ln", [2, MIX])
        self.wsT_d = din("gmlp_wsT", [128, 12, 128])
        self.bs_d = din("gmlp_bs", [12, 128])
        self.rt_d = din("moe_router", [D, 8])
        self.mgu_d = din("moe_w_gu", [8, D, 2 * DFF])
        self.mdn_d = din("moe_w_down", [8, DFF, D])
        self.y_d = nc.dram_tensor("y", [T, D], F32, kind="ExternalOutput").ap()
        self.mixT_d = nc.dram_tensor("mixT_scr", [D, T], BF16, kind="Internal").ap()

    def MM(self, out, lhsT, rhs, R, W, start=True, stop=True):
        n = rhs.free_size()
        self.P.op("pe", lambda h: h.matmul(out, lhsT, rhs, start=start, stop=stop), R, W, cost=max(64, n) / 1.9 + 8)

    def TR(self, out, in_, ident, R, W):
        self.P.op("pe", lambda h: h.transpose(out, in_, ident), R, W, cost=90.0)

    def ACT(self, out, in_, func, R, W, bias=None, scale=1.0, accum=None):
        kw = {}
        if bias is not None:
            kw["bias"] = bias
        if accum is not None:
            kw["accum_out"] = accum
        self.P.op("act", lambda h: h.activation(out, in_, func, scale=scale, **kw), R, W, cost=260 + out.free_size() / 1.2)

    def ecost(self, eng, out):
        n = out.free_size()
        return (120 + n / 0.96) if eng == "dve" else ((260 + n / 1.2) if eng == "act" else (200 + n / 0.45))

    def TT(self, eng, out, a, b, op, R, W):
        self.P.op(eng, lambda h: h.tensor_tensor(out, a, b, op), R, W, cost=self.ecost(eng, out))

    def TS(self, eng, out, a, s1, s2, op0, op1, R, W):
        if s2 is None:
            self.P.op(eng, lambda h: h.tensor_scalar(out, a, s1, None, op0), R, W, cost=self.ecost(eng, out))
        else:
            self.P.op(eng, lambda h: h.tensor_scalar(out, a, s1, s2, op0, op1), R, W, cost=self.ecost(eng, out))

    def STT(self, eng, out, a, s, b, op0, op1, R, W):
        self.P.op(eng, lambda h: h.scalar_tensor_tensor(out, a, s, b, op0, op1), R, W, cost=self.ecost(eng, out))

    def CP(self, eng, out, in_, R, W):
        if eng == "act":
            self.P.op(eng, lambda h: h.copy(out, in_), R, W, cost=self.ecost(eng, out))
        else:
            self.P.op(eng, lambda h: h.tensor_copy(out, in_), R, W, cost=self.ecost(eng, out))

    def MS(self, eng, out, val, W):
        self.P.op(eng, lambda h: h.memset(out, val), (), W)

    def RECIP(self, out, in_, R, W):
        o = self.P.op("dve", lambda h: h.reciprocal(out, in_), R, W, cost=120 + out.free_size() * 4.5)
        o.pin = "recip" in PINS

    def LOAD(self, q, lane, out, in_, W, R=()):
        o = self.P.dma(q, lane, lambda h: h.dma_start(out=out, in_=in_), R, W)
        o.cost = out.free_size() * 128 * 2 / 180.0

    def dump(self, name, ap, shape, R, dtype=F32):
        if name not in self.dbg:
            return
        d = self.nc.dram_tensor("dbg_" + name, shape, dtype, kind="ExternalOutput").ap()
        self.dbg_outs[name] = d
        l = self.P.lane("dbg_" + name)
        self.dbg_lanes.append(l)
        self.P.dma("sp", l, lambda h: h.dma_start(out=d, in_=ap), R, ())

    def build(self):
        nc, P = self.nc, self.P
        with contextlib.ExitStack() as st:
            NW = 52800
            big = st.enter_context(nc.sbuf_tensor("arena", [128, NW], F32))
            self.A = A = Arena(big, NW)
            self.ps = st.enter_context(nc.psum_tensor("ps", [128, 4096], F32))
            self.psb = [P.buf(f"psum{i}") for i in range(16)]
            self.out_lanes = []
            self.consts()
            self.mem_prep()
            if self.stage >= 1:
                self.layer0_mixer()
            if self.stage >= 2:
                self.resid_from_scratch(0)
            if self.stage >= 3:
                self.ffn_phase(0)
            if self.stage >= 4:
                self.layer1_mixer()
            if self.stage >= 5:
                self.ffn_phase(1)
            if self.stage >= 2:
                self.final_out()
            P.emit(final_lanes=self.out_lanes + self.dbg_lanes)
        return nc

    def bank(self, b):
        return self.ps[:, b * 512:(b + 1) * 512]

    def bankb(self, b):
        return [self.psb[2 * b], self.psb[2 * b + 1]]

    def consts(self):
        A, P = self.A, self.P
        self.identb = A.alloc([128], BF16)
        self.identf = A.alloc([128], F32)
        self.onesblk = A.alloc([128], BF16)
        self.blkf = A.alloc([128], F32)
        self.mAT = A.alloc([256], F32)
        self.mSL = A.alloc([128], F32)
        self.scanm = A.alloc([NB], F32)
        self.pc = A.alloc([64], F32)
        self.pc2 = A.alloc([32], F32)
        self.epsc = A.alloc([8], F32)
        self.bc = bc = P.buf("consts")
        l = self.lc = P.lane("const")
        self.MS("pool", self.identb, 0.0, [bc])
        P.op("pool", lambda h: h.affine_select(out=self.identb, in_=self.identb, pattern=[[-1, 128]],
                                               compare_op=ALU.not_equal, fill=1.0, base=0, channel_multiplier=1), [bc], [bc])
        self.CP("pool", self.identf, self.identb, [bc], [bc])
        self.MS("pool", self.onesblk, 0.0, [bc])
        self.MS("pool", self.onesblk[0:64, 0:64], 1.0, [bc])
        self.MS("pool", self.onesblk[64:128, 64:128], 1.0, [bc])
        self.CP("pool", self.blkf, self.onesblk, [bc], [bc])
        self.MS("pool", self.mAT, 0.0, [bc])
        P.op("pool", lambda h: h.affine_select(out=self.mAT[:, 0:128], in_=self.mAT[:, 0:128], pattern=[[-1, 128]],
                                               compare_op=ALU.is_ge, fill=1.0, base=0, channel_multiplier=1), [bc], [bc])
        P.op("pool", lambda h: h.affine_select(out=self.mAT[:, 128:256], in_=self.mAT[:, 128:256], pattern=[[-1, 128]],
                                               compare_op=ALU.is_gt, fill=1.0, base=0, channel_multiplier=1), [bc], [bc])
        self.MS("pool", self.mSL, 1.0, [bc])
        P.op("pool", lambda h: h.affine_select(out=self.mSL, in_=self.mSL, pattern=[[-1, 128]],
                                               compare_op=ALU.is_gt, fill=0.0, base=0, channel_multiplier=1), [bc], [bc])
        self.MS("pool", self.scanm, 1.0, [bc])
        self.MS("pool", self.scanm.rearrange("p (c t) -> p c t", t=128)[:, :, 0:1], 0.0, [bc])
        self.LOAD("sp", l, self.pc, self.pc_d[:, :], [bc])
        for i, v in enumerate([RMS_EPS, GN_EPS, LN_EPS, 1e-24, 0.0]):
            self.MS("pool", self.epsc[:, i:i + 1], v, [bc])
        self.TS("dve", self.pc2[:, 0:20], self.pc[:, 0:20], -1.0, 1.0, ALU.mult, ALU.add, [bc], [bc])
        self.TS("dve", self.pc2[:, 20:26], self.pc[:, 38:44], -1.0, 1.0, ALU.mult, ALU.add, [bc], [bc])

    def eps(self, i):
        return self.epsc[:, i:i + 1]

    def rms_tile(self, xs_ap, xs_b, gb_ap, gb_b, out_bf, out_b, sq_ap, st_ap, tmp_b, eng2="dve"):
        self.ACT(sq_ap, xs_ap, AF.Square, [xs_b], [tmp_b], accum=st_ap[:, 0:1])
        self.ACT(st_ap[:, 1:2], st_ap[:, 0:1], AF.Sqrt, [tmp_b], [tmp_b], bias=self.eps(0), scale=1.0 / D)
        self.RECIP(st_ap[:, 2:3], st_ap[:, 1:2], [tmp_b], [tmp_b])
        self.STT(eng2, out_bf, xs_ap, st_ap[:, 2:3], gb_ap, ALU.mult, ALU.mult, [xs_b, gb_b, tmp_b], [out_b])

    def mem_prep(self):
        A, P = self.A, self.P
        self.memT = A.alloc([8, 256], BF16)
        self.memT_b = P.buf("memT")
        A.mark()
        gb = A.alloc([D], F32)
        xs = A.alloc([D], F32)
        xn = A.alloc([D], BF16)
        sq = A.alloc([D], BF16)
        stt = A.alloc([4], F32)
        b_gb, b_xs, b_xn, b_t = P.buf(), P.buf(), P.buf(), P.buf()
        l1, l2 = P.lane("memg"), P.lane("memx")
        self.LOAD("sp", l1, gb, self.grow_d[0, :].partition_broadcast(128), [b_gb])
        for mt in range(2):
            self.LOAD("sp", l2, xs, self.mem_d[mt * 128:(mt + 1) * 128, :], [b_xs])
            self.rms_tile(xs, b_xs, gb, b_gb, xn, b_xn, sq, stt, b_t)
            pb = self.bank(mt).bitcast(BF16)
            for c in range(8):
                self.TR(pb[:, c * 128:(c + 1) * 128], xn[:, c * 128:(c + 1) * 128], self.identb, [b_xn, self.bc], self.bankb(mt))
            self.CP("act", self.memT[:, :, mt * 128:(mt + 1) * 128], pb.rearrange("p (c t) -> p c t", c=8), self.bankb(mt), [self.memT_b])
        self.dump("memT", self.memT, [128, 8, 256], [self.memT_b], BF16)
        self.mem_fence = P.fence()
        A.release()

    def kv_prep(self, li, kT, Vm, kv_b, wkv, wkv_b, lane):
        self.LOAD("pool", lane, wkv, self.wkv_d[li].rearrange("(c p) n -> p c n", p=128), [wkv_b])
        self.MS("pool", kT, 0.0, [kv_b])
        for j in range(2):
            pb = self.bank(j)
            for c in range(8):
                self.MM(pb[:, 0:256], wkv[:, c, j * 128:(j + 1) * 128], self.memT[:, c, :], [wkv_b, self.memT_b], self.bankb(j), start=(c == 0), stop=(c == 7))
            for hh in range(2):
                self.CP("act", kT[hh * 64:(hh + 1) * 64, 2 * j + hh, :], pb[hh * 64:(hh + 1) * 64, 0:256], self.bankb(j), [kv_b])
        for mc in range(2):
            pb = self.bank(2 + mc)
            for c in range(8):
                self.MM(pb[:, 0:256], self.memT[:, c, mc * 128:(mc + 1) * 128], wkv[:, c, 256:512], [wkv_b, self.memT_b], self.bankb(2 + mc), start=(c == 0), stop=(c == 7))
            self.CP("dve", Vm[:, mc, :], pb[:, 0:256], self.bankb(2 + mc), [kv_b])

    def attention_tile(self, qT, q_b, tsl, kT, Vm, kv_b, mixT_out, mix_b, pbanks, S):
        b0, b1 = pbanks
        sc = [self.bank(b0), self.bank(b1)]
        for h in range(4):
            j, hp = h // 2, (h % 2) * 64
            if ("odd" in self.skip and h % 2 == 1) or ("even" in self.skip and h % 2 == 0):
                continue
            self.MM(sc[j][:, (h % 2) * 256:(h % 2 + 1) * 256], qT[:, j, tsl], kT[:, h, :], [q_b, kv_b], self.bankb(pbanks[j]))
        mx, scs, pr, sm, prn, prT, tb = S["mx"], S["scs"], S["pr"], S["sm"], S["pr"], S["prT"], S["b"]
        if "att1" in self.skip:
            return
        for j in range(2):
            v3 = sc[j].rearrange("p (h m) -> p h m", h=2)
            self.P.op("dve", lambda h, v3=v3, j=j: h.tensor_reduce(mx[:, 2 * j:2 * j + 2], v3, AX.X, ALU.max), self.bankb(pbanks[j]), [tb]).pin = "reduce" in PINS
            self.TT("dve", scs[:, 2 * j:2 * j + 2, :], v3, mx[:, 2 * j:2 * j + 2].unsqueeze(2).to_broadcast([128, 2, 256]), ALU.subtract, self.bankb(pbanks[j]) + [tb], [tb])
        if "att2" in self.skip:
            return
        self.ACT(pr, scs, AF.Exp, [tb], [tb], scale=0.125)
        self.P.op("dve", lambda h: h.tensor_reduce(sm, pr, AX.X, ALU.add), [tb], [tb]).pin = "reduce" in PINS
        self.RECIP(sm, sm, [tb], [tb])
        self.TT("dve", prn, pr, sm.unsqueeze(2).to_broadcast([128, 4, 256]), ALU.mult, [tb], [tb])
        if "att3" in self.skip:
            return
        pbT = self.bank(b0).bitcast(BF16)
        for h in range(4):
            for mc in range(2):
                self.TR(pbT[:, (h * 2 + mc) * 128:(h * 2 + mc + 1) * 128], prn[:, h, mc * 128:(mc + 1) * 128], self.identb, [tb, self.bc], self.bankb(b0))
        self.CP("act", prT, pbT.rearrange("p (a t) -> p a t", a=8), self.bankb(b0), [tb])
        if "att4" in self.skip:
            return
        po = self.bank(b1)
        for h in range(4):
            j, hp = h // 2, (h % 2) * 64
            for mc in range(2):
                self.MM(po[hp:hp + 64, j * 128:(j + 1) * 128], Vm[:, mc, h * 64:(h + 1) * 64], prT[:, h * 2 + mc, :], [kv_b, tb], self.bankb(b1), start=(mc == 0), stop=(mc == 1))
        self.CP("dve", mixT_out, po[:, 0:256].rearrange("p (j t) -> p j t", j=2), self.bankb(b1), [mix_b])

    def attn_scratch(self):
        A = self.A
        return dict(mx=A.alloc([4], F32), scs=A.alloc([4, 256], F32), pr=A.alloc([4, 256], BF16), sm=A.alloc([4], F32),
                    prT=A.alloc([8, 128], BF16), b=self.P.buf("attn_s"))

    def layer0_mixer(self):
        A, P = self.A, self.P
        A.mark()
        fence0 = self.mem_fence
        nb = lambda n=None: P.buf(n, after=fence0)
        pc, pc2 = self.pc, self.pc2
        kT = A.alloc([4, 256], BF16); Vm = A.alloc([2, 256], BF16); b_kv = nb("kv")
        A.mark()
        wkv = A.alloc([8, 512], BF16); b_wkv = nb("wkv")
        lkv = P.lane("wkv0")
        self.kv_prep(0, kT, Vm, b_kv, wkv, b_wkv, lkv)
        A.release()
        fence0 = P.fence()
        win = A.alloc([8, 2816], BF16); b_win = nb("win")
        w2z = A.alloc([MIX], BF16)
        a2z = A.alloc([MIX], BF16)
        g2 = A.alloc([MIX], BF16); b_lora = nb("lora")
        gb = A.alloc([D], F32); b_gb = nb("gb")
        lw = [P.lane(f"w0_{i}") for i in range(3)]
        self.LOAD("sp", lw[0], gb, self.grow_d[1, :].partition_broadcast(128), [b_gb])
        for c in range(8):
            self.LOAD("pool", lw[1], win[:, c, :], self.win0_d[c * 128:(c + 1) * 128, :], [b_win])
        self.MS("pool", w2z[64:128, :], 0.0, [b_lora])
        self.MS("pool", a2z[0:64, :], 0.0, [b_lora])
        self.LOAD("pool", lw[2], w2z[0:64, :], self.w2_d[:, :], [b_lora])
        self.LOAD("pool", lw[2], a2z[64:128, :], self.a2_d[:, :], [b_lora])
        self.LOAD("pool", lw[2], g2, self.g2_d[:, :], [b_lora])
        xs1 = A.alloc([D], F32); xs = [xs1, xs1]; bxs1 = nb(); b_xs = [bxs1, bxs1]
        xn1 = A.alloc([D], BF16); xn = [xn1, xn1]; bxn1 = nb(); b_xn = [bxn1, bxn1]
        sqj = A.alloc([D], BF16); stt = [A.alloc([4], F32) for _ in range(2)]; b_nt = [nb() for _ in range(2)]
        lx1 = P.lane("x0_0"); lx = [lx1, lx1]
        nT = [A.alloc([8, NB + 1], BF16) for _ in range(2)]; b_nT = [nb() for _ in range(2)]
        pm = A.alloc([20, NB], F32); b_pm = [nb(f"pm{i}") for i in range(20)]
        ltmp = [A.alloc([NB], F32) for _ in range(3)]; b_ltmp = [nb() for _ in range(3)]
        qT = [A.alloc([2, NB], BF16) for _ in range(2)]; b_qT = [nb() for _ in range(2)]
        shp = [A.alloc([2, NB], BF16) for _ in range(2)]; b_shp = [nb() for _ in range(2)]
        mixT = [A.alloc([8, NB], BF16) for _ in range(2)]; b_mix = [[nb() for _ in range(8)] for _ in range(2)]
        lmix = [P.lane(f"mix{i}") for i in range(2)]
        AS = self.attn_scratch()
        NF = 12
        ft = [A.alloc([NB], F32) for _ in range(NF)]; b_ft = [nb() for _ in range(NF)]
        bt = [A.alloc([NB], BF16) for _ in range(2)]; b_bt = [nb() for _ in range(2)]
        def slot_tiles():
            d = {}
            d["gate"] = A.alloc([NB], F32); d["bonus"] = A.alloc([NB], F32); d["gC"] = A.alloc([2], F32)
            d["Kq"] = A.alloc([NB], BF16); d["Bq"] = A.alloc([NB], BF16); d["BgC"] = A.alloc([NB], BF16)
            d["KgC"] = A.alloc([NB], BF16); d["Vb"] = A.alloc([NB], BF16); d["KRz"] = A.alloc([2, 2, 2, 128], BF16); d["Kp"] = A.alloc([NB], BF16)
            d["RgF"] = A.alloc([NB], F32)
            d["tok"] = A.alloc([8, 128], BF16)
            oAT = [A.off, A.off + 512]
            d["AT1"] = A.alloc([4, 256], BF16); d["AT2"] = A.alloc([4, 256], BF16); d["Aab"] = A.alloc([4, 128], BF16)
            oX = [A.off, A.off + 256]; d["X"] = [A.alloc([4, 128], BF16) for _ in range(2)]
            oXT = [A.off, A.off + 256]; d["XT"] = [A.alloc([4, 128], BF16) for _ in range(2)]
            d["PT"] = [A.alloc([4, 128], BF16) for _ in range(2)]
            d["Nc"] = A.alloc([2, 64], F32)
            d["b_in"] = nb(); self.MS("pool", d["KRz"], 0.0, [d["b_in"]]); d["b_tok"] = nb(); d["b_A"] = nb(); d["b_X"] = [nb(), nb()]; d["b_XT"] = [nb(), nb()]; d["b_PT"] = [nb(), nb()]
            d["AV"] = A.view(oX[0], [4, 64], BF16); d["b_AV"] = d["b_X"][0]
            d["yb"] = A.view(oX[1], [NB], BF16)
            d["WU"] = A.view(oXT[0], [2, 2, 2, 64], BF16); d["b_WU"] = d["b_XT"][0]
            d["McT"] = A.view(oXT[1], [2, 128], BF16); d["QcT"] = A.view(oXT[1] + 128, [2, 128], BF16)
            d["b_MN"] = d["b_XT"][1]; d["b_Q"] = d["b_XT"][1]
            d["b_Nc"] = nb()
            d["y"] = A.view(oAT[0], [NB], F32); d["yc"] = A.view(oAT[0] + 256, [NB], F32); d["rs"] = A.view(oAT[1], [NB], F32)
            d["b_y"] = d["b_A"]
            return d
        NSLOT = 3
        SL = [slot_tiles() for _ in range(NSLOT)]
        Sf = A.alloc([6, 64], F32); Sb = A.alloc([6, 64], BF16); Sbd = A.alloc([6, 128], BF16); b_S = [nb(f"S{i}") for i in range(6)]
        identb4 = self.identb.unsqueeze(1).to_broadcast([128, 4, 128])
        for cc in range(6):
            self.MS("pool", Sf[:, cc, :], 0.0, [b_S[cc]])
            self.MS("pool", Sb[:, cc, :], 0.0, [b_S[cc]])
            self.MS("pool", Sbd[:, cc, :], 0.0, [b_S[cc]])

        psum_rot = {"f": [0, 1], "u0": [2, 3], "u1": [4, 5], "u2": [6, 7]}
        rot_idx = {"f": 0, "u0": 0, "u1": 0, "u2": 0}

        def nextbank(key):
            lst = psum_rot[key]
            b = lst[rot_idx[key] % len(lst)]
            rot_idx[key] += 1
            return b

        def front(bi):
            s = bi % 2
            t0 = bi * NB
            for tt in range(2):
                k = tt
                self.LOAD("sp", lx[k], xs[k], self.x_d[t0 + tt * 128:t0 + (tt + 1) * 128, :], [b_xs[k]])
                self.rms_tile(xs[k], b_xs[k], gb, b_gb, xn[k], b_xn[k], sqj, stt[k], b_nt[k])
                b = nextbank("f")
                pb = self.bank(b).bitcast(BF16)
                for c in range(8):
                    self.TR(pb[:, c * 128:(c + 1) * 128], xn[k][:, c * 128:(c + 1) * 128], self.identb, [b_xn[k], self.bc], self.bankb(b))
                self.CP("act" if tt == 0 else "dve", nT[s][:, :, 1 + tt * 128:1 + (tt + 1) * 128], pb.rearrange("p (c t) -> p c t", c=8), self.bankb(b), [b_nT[s]])
                yield
            yield ("wait", f"pmfree{bi}")
            if bi == 0:
                self.MS("pool", nT[s][:, :, 0:1], 0.0, [b_nT[s]])
            else:
                self.CP("pool", nT[s][:, :, 0:1], nT[1 - s][:, :, NB:NB + 1], [b_nT[1 - s]], [b_nT[s]])
            for fc in range(22):
                if "inproj" in self.skip:
                    continue
                b = nextbank("f")
                pb = self.bank(b)
                if fc < 20:
                    for c in range(8):
                        self.MM(pb[:, 0:NB + 1], win[:, c, fc * 128:(fc + 1) * 128], nT[s][:, c, 0:NB + 1], [b_win, b_nT[s]], self.bankb(b), start=(c == 0), stop=(c == 7))
                    k = fc % 3
                    self.ACT(ltmp[k], pb[:, 0:NB], AF.Copy, self.bankb(b) + [self.bc], [b_ltmp[k]], scale=pc[:, fc:fc + 1])
                    self.STT("dve", pm[:, fc, :], pb[:, 1:NB + 1], pc2[:, fc:fc + 1], ltmp[k], ALU.mult, ALU.add, self.bankb(b) + [b_ltmp[k], self.bc], [b_pm[fc]])
                else:
                    j = fc - 20
                    for c in range(8):
                        self.MM(pb[:, 0:NB], win[:, c, fc * 128:(fc + 1) * 128], nT[s][:, c, 1:NB + 1], [b_win, b_nT[s]], self.bankb(b), start=(c == 0), stop=(c == 7))
                    self.CP("act", qT[s][:, j, :], pb[:, 0:NB], self.bankb(b), [b_qT[s]])
                yield
            if "shp" in self.skip:
                return
            self.ACT(shp[s][0:64, 0, :], pm[0:64, 18, :], AF.Tanh, [b_pm[18]], [b_shp[s]])
            self.CP("pool", shp[s][64:128, 0, :], pm[64:128, 18, :], [b_pm[18]], [b_shp[s]])
            self.ACT(shp[s][:, 1, :], pm[:, 19, :], AF.Sigmoid, [b_pm[19]], [b_shp[s]])
            yield

        def prep(bi, cc, sl):
            s = bi % 2
            d = SL[sl]
            key = f"u{sl}"
            r, k, v = pm[:, cc, :], pm[:, 6 + cc, :], pm[:, 12 + cc, :]
            br, bk, bv = b_pm[cc], b_pm[6 + cc], b_pm[12 + cc]
            cs_ = slice(cc * 128, (cc + 1) * 128)
            col = lambda base: pc[:, base + cc:base + cc + 1]
            f_lw, f_cs, f_csp, f_g, f_gi, f_gp, f_a, f_kk, f_rn, f_kn, f_b, f_gq, f_t = range(13)
            F = lambda i: ft[i]
            Bf = lambda i: b_ft[i]
            b1 = nextbank(key); p1 = self.bank(b1)
            self.MM(p1[:, 0:NB], w2z[:, cs_], shp[s][:, 0, :], [b_lora, b_shp[s]], [self.psb[2 * b1]])
            self.MM(p1[:, NB:2 * NB], a2z[:, cs_], shp[s][:, 0, :], [b_lora, b_shp[s]], [self.psb[2 * b1 + 1]])
            b2 = nextbank(key); p2 = self.bank(b2)
            self.MM(p2[:, 0:NB], g2[:, cs_], shp[s][:, 1, :], [b_lora, b_shp[s]], [self.psb[2 * b2]])
            self.ACT(F(f_lw), p1[:, 0:NB], AF.Sigmoid, [self.psb[2 * b1], self.bc], [Bf(f_lw)], bias=col(20))
            self.ACT(F(f_a), p1[:, NB:2 * NB], AF.Sigmoid, [self.psb[2 * b1 + 1], self.bc], [Bf(f_a)], bias=col(26))
            self.CP("act", d["gate"], p2[:, 0:NB], [self.psb[2 * b2]], [d["b_in"]])
            yield
            o_ = self.P.op("dve", lambda h: h.tensor_tensor_scan(F(f_cs), self.scanm, F(f_lw), 0.0, ALU.mult, ALU.add), [Bf(f_lw), self.bc], [Bf(f_cs)])
            o_.pin = "scan" in PINS
            self.TT("dve", F(f_csp), F(f_cs), F(f_lw), ALU.subtract, [Bf(f_cs), Bf(f_lw)], [Bf(f_csp)])
            self.ACT(F(f_g), F(f_cs), AF.Exp, [Bf(f_cs)], [Bf(f_g)], scale=-C0)
            self.ACT(F(f_gi), F(f_cs), AF.Exp, [Bf(f_cs)], [Bf(f_gi)], scale=C0)
            self.ACT(F(f_gp), F(f_csp), AF.Exp, [Bf(f_csp)], [Bf(f_gp)], scale=-C0)
            self.TS("dve", F(f_kk), k, col(32), None, ALU.mult, None, [bk, self.bc], [Bf(f_kk)])
            self.ACT(bt[0], F(f_kk), AF.Square, [Bf(f_kk)], [b_bt[0]])
            self.MM(p2[:, NB:2 * NB], self.onesblk, bt[0], [self.bc, b_bt[0]], [self.psb[2 * b2 + 1]])
            self.ACT(F(f_rn), p2[:, NB:2 * NB], AF.Sqrt, [self.psb[2 * b2 + 1], self.bc], [Bf(f_rn)], bias=self.eps(3))
            self.RECIP(F(f_rn), F(f_rn), [Bf(f_rn)], [Bf(f_rn)])
            self.TT("dve", F(f_kk), F(f_kk), F(f_rn), ALU.mult, [Bf(f_kk), Bf(f_rn)], [Bf(f_kk)])
            yield
            self.TS("dve", F(f_kn), F(f_a), col(38), pc2[:, 20 + cc:21 + cc], ALU.mult, ALU.add, [Bf(f_a), self.bc], [Bf(f_kn)])
            self.TT("dve", F(f_kn), F(f_kn), k, ALU.mult, [Bf(f_kn), bk], [Bf(f_kn)])
            self.TT("pool", F(f_b), F(f_kk), F(f_a), ALU.mult, [Bf(f_kk), Bf(f_a)], [Bf(f_b)])
            self.STT("dve", bt[1], r, col(44), F(f_kn), ALU.mult, ALU.mult, [br, Bf(f_kn), self.bc], [b_bt[1]])
            b3 = nextbank(key); p3 = self.bank(b3)
            self.MM(p3[:, 0:NB], self.onesblk, bt[1], [self.bc, b_bt[1]], [self.psb[2 * b3]])
            self.TT("dve", d["bonus"], p3[:, 0:NB], v, ALU.mult, [self.psb[2 * b3], bv], [d["b_in"]])
            yield
            KRz = d["KRz"]
            c2 = lambda ap: ap.rearrange("p (c t) -> p c t", c=2)
            self.TT("pool", d["Kp"], F(f_kk), F(f_gp), ALU.mult, [Bf(f_kk), Bf(f_gp)], [d["b_in"]])
            for hh in range(2):
                hsl = slice(hh * 64, (hh + 1) * 64)
                self.TT("pool", KRz[hsl, hh, :, 0, :], c2(F(f_kk)[hsl]), c2(F(f_gp)[hsl]), ALU.mult, [Bf(f_kk), Bf(f_gp)], [d["b_in"]])
                self.TT("pool", KRz[hsl, hh, :, 1, :], c2(r[hsl]), c2(F(f_g)[hsl]), ALU.mult, [br, Bf(f_g)], [d["b_in"]])
            for ch in range(2):
                tsl = slice(ch * 128, (ch + 1) * 128)
                self.TS("dve", F(f_gq)[:, tsl], F(f_gi)[:, tsl], F(f_g)[:, ch * 128 + 127:ch * 128 + 128], None, ALU.mult, None, [Bf(f_gi), Bf(f_g)], [Bf(f_gq)])
                self.CP("pool", d["gC"][:, ch:ch + 1], F(f_g)[:, ch * 128 + 127:ch * 128 + 128], [Bf(f_g)], [d["b_in"]])
            self.TT("dve", d["RgF"], r, F(f_g), ALU.mult, [br, Bf(f_g)], [d["b_in"]])
            self.TT("dve", d["Kq"], F(f_kn), F(f_gi), ALU.mult, [Bf(f_kn), Bf(f_gi)], [d["b_in"]])
            self.TT("pool", d["Bq"], F(f_b), F(f_gi), ALU.mult, [Bf(f_b), Bf(f_gi)], [d["b_in"]])
            self.TT("dve", d["KgC"], F(f_kn), F(f_gq), ALU.mult, [Bf(f_kn), Bf(f_gq)], [d["b_in"]])
            self.TT("pool", d["BgC"], F(f_b), F(f_gq), ALU.mult, [Bf(f_b), Bf(f_gq)], [d["b_in"]])
            self.CP("act", d["Vb"], v, [bv], [d["b_in"]])
            yield

        def algo(bi, cc, sl):
            s = bi % 2
            d = SL[sl]
            key = f"u{sl}"
            bin_ = d["b_in"]
            KRz, Kq, Bq, tok = d["KRz"], d["Kq"], d["Bq"], d["tok"]
            hs = lambda hh: slice(hh * 64, (hh + 1) * 64)
            b = nextbank(key); pb = self.bank(b).bitcast(BF16)
            for ch in range(2):
                tsl = slice(ch * 128, (ch + 1) * 128)
                srcs = [d["Vb"][:, tsl], d["Kp"][:, tsl], d["BgC"][:, tsl], d["KgC"][:, tsl]]
                for q in range(4):
                    i = ch * 4 + q
                    self.TR(pb[:, i * 128:(i + 1) * 128], srcs[q], self.identb, [bin_, self.bc], self.bankb(b))
            self.CP("act", tok, pb.rearrange("p (a t) -> p a t", a=8), self.bankb(b), [d["b_tok"]])
            yield
            mATb = self.mAT.unsqueeze(1).to_broadcast([128, 2, 256])
            for which, L, dst in ((0, Bq, d["AT1"]), (1, Kq, d["AT2"])):
                for ch in range(2):
                    b = nextbank(key); pb = self.bank(b)
                    for hh in range(2):
                        self.MM(pb[:, hh * 256:(hh + 1) * 256], L[:, ch * 128:(ch + 1) * 128], KRz[:, hh, ch].rearrange("p a t -> p (a t)"), [bin_], self.bankb(b))
                    self.TT("dve", dst[:, ch * 2:ch * 2 + 2, :], pb.rearrange("p (u x) -> p u x", u=2), mATb, ALU.mult, self.bankb(b) + [self.bc], [d["b_A"]])
                yield
            b = nextbank(key); pb = self.bank(b)
            for ch in range(2):
                for hh in range(2):
                    u = ch * 2 + hh
                    self.MM(pb[:, u * 128:(u + 1) * 128], KRz[:, hh, ch, 0, :], Bq[:, ch * 128:(ch + 1) * 128], [bin_], self.bankb(b))
            self.TT("dve", d["Aab"], pb.rearrange("p (u x) -> p u x", u=4), self.mSL.unsqueeze(1).to_broadcast([128, 4, 128]), ALU.mult, self.bankb(b) + [self.bc], [d["b_A"]])
            AabT = d["AT1"][:, :, 0:128]
            ArbT = d["AT1"][:, :, 128:256]
            AakT = d["AT2"][:, :, 0:128]
            ArkT = d["AT2"][:, :, 128:256]
            self.TT("pool", d["PT"][0], identb4, AabT, ALU.subtract, [self.bc, d["b_A"]], [d["b_PT"][0]])
            yield
            Xc, XTc, bX, bXT = d["Aab"], AabT, d["b_A"], d["b_A"]
            for lv in range(6):
                Xn, bXn = d["X"][lv % 2], d["b_X"][lv % 2]
                XTn, bXTn = d["XT"][lv % 2], d["b_XT"][lv % 2]
                b = nextbank(key); pb = self.bank(b)
                for u in range(4):
                    self.MM(pb[:, u * 128:(u + 1) * 128], XTc[:, u, :], Xc[:, u, :], [bX, bXT], self.bankb(b))
                self.CP("act", Xn, pb.rearrange("p (u x) -> p u x", u=4), self.bankb(b), [bXn])
                if lv < 5:
                    b = nextbank(key); pb2 = self.bank(b)
                    for u in range(4):
                        self.MM(pb2[:, u * 128:(u + 1) * 128], Xc[:, u, :], XTc[:, u, :], [bX, bXT], self.bankb(b))
                    self.CP("dve", XTn, pb2.rearrange("p (u x) -> p u x", u=4), self.bankb(b), [bXTn])
                yield
                PTo, bPo = d["PT"][lv % 2], d["b_PT"][lv % 2]
                PTn, bPn = d["PT"][(lv + 1) % 2], d["b_PT"][(lv + 1) % 2]
                b = nextbank(key); pb3 = self.bank(b)
                for u in range(4):
                    self.MM(pb3[:, u * 128:(u + 1) * 128], Xn[:, u, :], PTo[:, u, :], [bXn, bPo], self.bankb(b))
                self.TT("dve", PTn, pb3.rearrange("p (u x) -> p u x", u=4), PTo, ALU.add, self.bankb(b) + [bPo], [bPn])
                Xc, XTc, bX, bXT = Xn, XTn, bXn, bXTn
                yield
            TinvT, bT = d["PT"][0], d["b_PT"][0]
            b = nextbank(key); pb = self.bank(b)
            for ch in range(2):
                for hh in range(2):
                    u = ch * 2 + hh
                    self.MM(pb[:, u * 64:(u + 1) * 64], AakT[:, u, :], tok[:, ch * 4 + 0, hs(hh)], [d["b_A"], d["b_tok"]], [self.psb[2 * b]])
            self.CP("act", d["AV"], pb[:, 0:256].rearrange("p (u x) -> p u x", u=4), [self.psb[2 * b]], [d["b_AV"]])
            yield
            b = nextbank(key); pb = self.bank(b)
            pbv = pb.rearrange("p (c w h x) -> p c w h x", c=2, w=2, h=2)
            for ch in range(2):
                for hh in range(2):
                    u = ch * 2 + hh
                    self.MM(pbv[:, ch, 0, hh, :], TinvT[:, u, :], tok[:, ch * 4 + 1, hs(hh)], [bT, d["b_tok"]], self.bankb(b))
                    self.MM(pbv[:, ch, 1, hh, :], TinvT[:, u, :], d["AV"][:, u, :], [bT, d["b_AV"]], self.bankb(b))
            self.ACT(d["WU"].rearrange("p c w h x -> p (c w h x)"), pb, AF.Copy, self.bankb(b), [d["b_WU"]], scale=-1.0)
            yield
            WU = d["WU"]
            b = nextbank(key); pb = self.bank(b)
            for ch in range(2):
                Wn = WU[:, ch, 0].rearrange("p h x -> p (h x)")
                Un = WU[:, ch, 1].rearrange("p h x -> p (h x)")
                self.MM(pb[:, ch * 128:(ch + 1) * 128], Wn, tok[:, ch * 4 + 2, :], [d["b_WU"], d["b_tok"]], [self.psb[2 * b]])
                self.MM(pb[:, 256 + ch * 128:256 + (ch + 1) * 128], tok[:, ch * 4 + 3, :], tok[:, ch * 4 + 0, :], [d["b_tok"]], [self.psb[2 * b + 1]], start=True, stop=False)
                self.MM(pb[:, 256 + ch * 128:256 + (ch + 1) * 128], tok[:, ch * 4 + 2, :], Un, [d["b_tok"], d["b_WU"]], [self.psb[2 * b + 1]], start=False, stop=True)
            self.TT("dve", d["McT"], pb[:, 0:256].rearrange("p (c x) -> p c x", c=2), self.blkf.unsqueeze(1).to_broadcast([128, 2, 128]), ALU.mult, [self.psb[2 * b], self.bc], [d["b_MN"]])
            for ch in range(2):
                for hh in range(2):
                    self.CP("dve" if hh == 0 else "act", d["Nc"][hs(hh), ch, :], pb[hs(hh), 256 + ch * 128 + hh * 64:256 + ch * 128 + (hh + 1) * 64], [self.psb[2 * b + 1]], [d["b_Nc"]])
            b = nextbank(key); pq = self.bank(b)
            for ch in range(2):
                for hh in range(2):
                    u = ch * 2 + hh
                    self.MM(pq[hs(hh), ch * 128:(ch + 1) * 128], WU[:, ch, 0, hh, :], ArbT[:, u, :], [d["b_WU"], d["b_A"]], [self.psb[2 * b]])
            self.TT("dve", d["QcT"], pq[:, 0:256].rearrange("p (c x) -> p c x", c=2), d["RgF"].rearrange("p (c x) -> p c x", c=2), ALU.add, [self.psb[2 * b], bin_], [d["b_Q"]])
            yield
            b = nextbank(key); py = self.bank(b)
            for ch in range(2):
                self.MM(py[:, ch * 128:(ch + 1) * 128], Sbd[:, cc, :], d["QcT"][:, ch, :], [b_S[cc], d["b_Q"]], [self.psb[2 * b]], start=True, stop=False)
                for hh in range(2):
                    u = ch * 2 + hh
                    o = py[hs(hh), ch * 128:(ch + 1) * 128]
                    self.MM(o, tok[:, ch * 4 + 0, hs(hh)], ArkT[:, u, :], [d["b_tok"], d["b_A"]], [self.psb[2 * b]], start=False, stop=False)
                    self.MM(o, WU[:, ch, 1, hh, :], ArbT[:, u, :], [d["b_WU"], d["b_A"]], [self.psb[2 * b]], start=False, stop=(hh == 1))
                pst = py[:, 256 + ch * 64:256 + (ch + 1) * 64]
                self.MM(pst, d["McT"][:, ch, :], Sb[:, cc, :], [d["b_MN"], b_S[cc]], [self.psb[2 * b + 1]])
                self.STT("dve", Sf[:, cc, :], Sf[:, cc, :], d["gC"][:, ch:ch + 1], pst, ALU.mult, ALU.add, [self.psb[2 * b + 1], bin_, b_S[cc]], [b_S[cc]])
                self.TT("dve", Sf[:, cc, :], Sf[:, cc, :], d["Nc"][:, ch, :], ALU.add, [d["b_Nc"], b_S[cc]], [b_S[cc]])
                self.CP("dve", Sb[:, cc, :], Sf[:, cc, :], [b_S[cc]], [b_S[cc]])
                for hh in range(2):
                    self.CP("pool", Sbd[hs(hh), cc, hh * 64:(hh + 1) * 64], Sf[hs(hh), cc, :], [b_S[cc]], [b_S[cc]])
                yield
            y, yb, yc, rs = d["y"], d["yb"], d["yc"], d["rs"]
            by = d["b_y"]
            self.CP("dve", y, py[:, 0:256], [self.psb[2 * b]], [by])
            byb = d["b_X"][1]
            self.CP("act", yb, py[:, 0:256], [self.psb[2 * b]], [byb])
            b2 = nextbank(key); pm_ = self.bank(b2)
            self.MM(pm_[:, 0:256], self.onesblk, yb, [self.bc, byb], [self.psb[2 * b2]])
            self.STT("dve", yc, pm_[:, 0:256], -1.0 / 64, y, ALU.mult, ALU.add, [self.psb[2 * b2], by], [by])
            self.ACT(yb, yc, AF.Square, [by], [byb])
            self.MM(pm_[:, 256:512], self.onesblk, yb, [self.bc, byb], [self.psb[2 * b2 + 1]])
            self.ACT(rs, pm_[:, 256:512], AF.Sqrt, [self.psb[2 * b2 + 1], self.bc], [by], bias=self.eps(1), scale=1.0 / 64)
            self.RECIP(rs, rs, [by], [by])
            self.TT("dve", yc, yc, rs, ALU.mult, [by], [by])
            self.TS("dve", yc, yc, col_lnw(cc), col_lnb(cc), ALU.mult, ALU.add, [by, self.bc], [by])
            self.TT("pool", yc, yc, d["bonus"], ALU.add, [by, bin_], [by])
            self.TT("dve", mixT[s][:, cc, :], yc, d["gate"], ALU.mult, [by, bin_], [b_mix[s][cc]])
            yield

        col_lnw = lambda cc: pc[:, 50 + cc:51 + cc]
        col_lnb = lambda cc: pc[:, 56 + cc:57 + cc]

        def unit(bi, cc, sl):
            s = bi % 2
            yield ("wait", f"pmready{bi}")
            yield ("acq", f"slot{sl}")
            yield ("acq", "ft")
            if cc == 0:
                yield ("acq", f"mix{s}")
            yield from prep(bi, cc, sl)
            if self.alg < 1:
                yield ("rel", "ft")
                if cc == 5:
                    yield ("set", f"pmfree{bi + 1}")
                yield ("rel", f"slot{sl}")
                yield ("set", f"udone{bi}_{cc}")
                return
            yield ("rel", "ft")
            if cc == 5:
                yield ("set", f"pmfree{bi + 1}")
            yield from algo(bi, cc, sl)
            yield ("rel", f"slot{sl}")
            yield ("set", f"udone{bi}_{cc}")

        def attn(bi):
            s = bi % 2
            yield ("wait", f"pmready{bi}")
            for tt in range(2):
                if "attn" in self.skip:
                    continue
                self.attention_tile(qT[s], b_qT[s], slice(tt * 128, (tt + 1) * 128), kT, Vm, b_kv,
                                    mixT[s][:, 6:8, tt * 128:(tt + 1) * 128], b_mix[s][6 + tt % 2], (0, 1), AS)
                yield
            for cc in range(6):
                yield ("wait", f"udone{bi}_{cc}")
            t0 = bi * NB
            if "store" not in self.skip:
              self.P.dma("sp", lmix[s], lambda h: h.dma_start(out=self.mixT_d.rearrange("(c p) t -> p c t", p=128)[:, :, t0:t0 + NB], in_=mixT[s]),
                       [bb for bb in b_mix[s]], [self.b_mixscr[bi]])
            yield ("rel", f"mix{s}")

        def frontw(bi):
            yield from front(bi)
            yield ("set", f"pmready{bi}")

        self.b_mixscr = [P.buf(f"mixscr{i}") for i in range(NBLK)]
        gens = [frontw(0)]
        u = 0
        pre = ["pmfree0"]
        for bi in range(self.nblk):
            for cc in range(6):
                if cc < self.ncc:
                    gens.append(unit(bi, cc, u % NSLOT))
                    u += 1
                else:
                    pre.append(f"udone{bi}_{cc}")
                    if cc == 5:
                        pre.append(f"pmfree{bi + 1}")
            if bi + 1 < self.nblk:
                gens.append(frontw(bi + 1))
            gens.append(attn(bi))
        drive(gens, 6, preset=pre)
        if "pm" in self.dbg:
            self.dump("pm", pm, [128, 20, NB], b_pm)
        self.dump("mixT", mixT[(self.nblk - 1) % 2], [128, 8, NB], [bb for bb in b_mix[(self.nblk - 1) % 2]], BF16)
        self.l0_fence = P.fence()
        A.release()

    def resid_from_scratch(self, li):
        A, P = self.A, self.P
        fence = P.fence()
        nb = lambda n=None: P.buf(n, after=fence)
        self.X = A.alloc([NT, D], F32)
        self.b_X = [nb(f"X{i}") for i in range(NT)]
        A.mark()
        wout = A.alloc([8, D], BF16); b_w = nb()
        l = P.lane("wout0")
        self.LOAD("pool", l, wout, self.wout_d[li].rearrange("(c p) n -> p c n", p=128), [b_w])
        mt = [A.alloc([8, 128], BF16) for _ in range(2)]; b_mt = [nb(), nb()]
        lm = [P.lane("mt0"), P.lane("mt1")]
        lxx = [P.lane("xx0"), P.lane("xx1")]
        scr = self.mixT_d.rearrange("(c p) t -> p c t", p=128)
        for tt in range(NT):
            k = tt % 2
            self.LOAD("sp", lm[k], mt[k], scr[:, :, tt * 128:(tt + 1) * 128], [b_mt[k]], R=[self.b_mixscr[tt // 2]])
            self.LOAD("sp", lxx[k], self.X[:, tt, :], self.x_d[tt * 128:(tt + 1) * 128, :], [self.b_X[tt]])
            for half in range(2):
                b = (tt * 2 + half) % 8
                pb = self.bank(b)
                for c in range(8):
                    self.MM(pb, mt[k][:, c, :], wout[:, c, half * 512:(half + 1) * 512], [b_mt[k], b_w], self.bankb(b), start=(c == 0), stop=(c == 7))
                self.TT("dve", self.X[:, tt, half * 512:(half + 1) * 512], pb, self.X[:, tt, half * 512:(half + 1) * 512], ALU.add, self.bankb(b) + [self.b_X[tt]], [self.b_X[tt]])
        self.dump("X0a", self.X, [128, NT, D], self.b_X)
        A.release()

    def final_out(self):
        A, P = self.A, self.P
        fence = P.fence()
        nb = lambda n=None: P.buf(n, after=fence)
        A.mark()
        gb = A.alloc([D], F32); b_gb = nb()
        l = P.lane("gfin")
        self.LOAD("sp", l, gb, self.grow_d[5, :].partition_broadcast(128), [b_gb])
        ot = [A.alloc([D], F32) for _ in range(2)]; b_ot = [nb(), nb()]
        sq = A.alloc([D], BF16); stt = [A.alloc([4], F32) for _ in range(2)]; b_t = [nb(), nb()]
        lo = [P.lane("out0"), P.lane("out1")]
        self.out_lanes += lo
        for tt in range(NT):
            k = tt % 2
            xs = self.X[:, tt, :]
            self.ACT(sq, xs, AF.Square, [self.b_X[tt]], [b_t[k]], accum=stt[k][:, 0:1])
            self.ACT(stt[k][:, 1:2], stt[k][:, 0:1], AF.Sqrt, [b_t[k], self.bc], [b_t[k]], bias=self.eps(0), scale=1.0 / D)
            self.RECIP(stt[k][:, 2:3], stt[k][:, 1:2], [b_t[k]], [b_t[k]])
            self.STT("dve", ot[k], xs, stt[k][:, 2:3], gb, ALU.mult, ALU.mult, [self.b_X[tt], b_gb, b_t[k]], [b_ot[k]])
            self.P.dma("sp", lo[k], lambda h, k=k, tt=tt: h.dma_start(out=self.y_d[tt * 128:(tt + 1) * 128, :], in_=ot[k]), [b_ot[k]], ())
        A.release()

    def ffn_phase(self, li):
        A, P = self.A, self.P
        fence = P.fence()
        nb = lambda n=None: P.buf(n, after=fence)
        A.mark()
        moe = (li == 1)
        nT2 = A.alloc([8, T], BF16); b_nT2 = [nb(f"nT2_{i}") for i in range(NT)]
        gb = A.alloc([D], F32); b_gb = nb()
        lg = P.lane(f"g2_{li}")
        self.LOAD("sp", lg, gb, self.grow_d[2 if li == 0 else 4, :].partition_broadcast(128), [b_gb])
        if moe:
            comb = A.alloc([NT, 8], F32); b_comb = [nb() for _ in range(NT)]
        A.mark()
        xnf = [A.alloc([D], F32) for _ in range(2)] if moe else None
        xn = [A.alloc([D], BF16) for _ in range(2)]; b_xn = [nb(), nb()]
        sq = A.alloc([D], BF16); stt = [A.alloc([4], F32) for _ in range(2)]; b_t = [nb(), nb()]
        if moe:
            wr = A.alloc([8, 8], F32); b_wr = nb()
            lr = P.lane("router")
            self.LOAD("sp", lr, wr, self.rt_d.rearrange("(c p) e -> p c e", p=128), [b_wr])
            nTf = [A.alloc([8, 128], F32) for _ in range(2)]; b_nTf = [nb(), nb()]
            rs_ = [A.alloc([64], F32) for _ in range(2)]; b_rs = [nb(), nb()]
        for tt in range(NT):
            k = tt % 2
            xs = self.X[:, tt, :]
            self.ACT(sq, xs, AF.Square, [self.b_X[tt]], [b_t[k]], accum=stt[k][:, 0:1])
            self.ACT(stt[k][:, 1:2], stt[k][:, 0:1], AF.Sqrt, [b_t[k], self.bc], [b_t[k]], bias=self.eps(0), scale=1.0 / D)
            self.RECIP(stt[k][:, 2:3], stt[k][:, 1:2], [b_t[k]], [b_t[k]])
            if moe:
                self.STT("dve", xnf[k], xs, stt[k][:, 2:3], gb, ALU.mult, ALU.mult, [self.b_X[tt], b_gb, b_t[k]], [b_xn[k]])
                self.CP("act", xn[k], xnf[k], [b_xn[k]], [b_xn[k]])
            else:
                self.STT("dve", xn[k], xs, stt[k][:, 2:3], gb, ALU.mult, ALU.mult, [self.b_X[tt], b_gb, b_t[k]], [b_xn[k]])
            b = tt % 2
            pb = self.bank(b).bitcast(BF16)
            for c in range(8):
                self.TR(pb[:, c * 128:(c + 1) * 128], xn[k][:, c * 128:(c + 1) * 128], self.identb, [b_xn[k], self.bc], self.bankb(b))
            self.CP("act" if tt % 2 == 0 else "dve", nT2[:, :, tt * 128:(tt + 1) * 128], pb.rearrange("p (c t) -> p c t", c=8), self.bankb(b), [b_nT2[tt]])
            if moe:
                pf = self.ps[:, 1024 + k * 1024:2048 + k * 1024]
                pfb = self.bankb(2 + 2 * k) + self.bankb(3 + 2 * k)
                for c in range(8):
                    self.TR(pf[:, c * 128:(c + 1) * 128], xnf[k][:, c * 128:(c + 1) * 128], self.identf, [b_xn[k], self.bc], pfb)
                self.CP("act", nTf[k], pf.rearrange("p (c t) -> p c t", c=8), pfb, [b_nTf[k]])
                pl = self.bank(6 + k)
                for c in range(8):
                    self.MM(pl[:, 0:8], nTf[k][:, c, :], wr[:, c, :], [b_nTf[k], b_wr], self.bankb(6 + k), start=(c == 0), stop=(c == 7))
                r = rs_[k]; br = b_rs[k]
                lgt, m1, eq1, l2, m2, eq2, dl, ee, g1, g2 = (r[:, 0:8], r[:, 8:9], r[:, 16:24], r[:, 24:32], r[:, 9:10], r[:, 32:40], r[:, 10:11], r[:, 11:12], r[:, 12:13], r[:, 13:14])
                self.CP("dve", lgt, pl[:, 0:8], self.bankb(6 + k), [br])
                P.op("dve", lambda h, m1=m1, lgt=lgt: h.tensor_reduce(m1, lgt, AX.X, ALU.max), [br], [br])
                self.TS("dve", eq1, lgt, m1, None, ALU.is_equal, None, [br], [br])
                self.STT("dve", l2, eq1, -1e30, lgt, ALU.mult, ALU.add, [br], [br])
                P.op("dve", lambda h, m2=m2, l2=l2: h.tensor_reduce(m2, l2, AX.X, ALU.max), [br], [br])
                self.TS("dve", eq2, l2, m2, None, ALU.is_equal, None, [br], [br])
                self.TT("dve", dl, m2, m1, ALU.subtract, [br], [br])
                self.ACT(ee, dl, AF.Exp, [br], [br])
                self.TS("dve", g1, ee, 1.0, None, ALU.add, None, [br], [br])
                self.RECIP(g1, g1, [br], [br])
                self.TT("dve", g2, ee, g1, ALU.mult, [br], [br])
                self.TS("dve", eq1, eq1, g1, None, ALU.mult, None, [br], [br])
                self.STT("dve", comb[:, tt, :], eq2, g2, eq1, ALU.mult, ALU.add, [br], [b_comb[tt]])
        if moe:
            self.dump("comb", comb, [128, NT, 8], b_comb)
        A.release()
        fence = P.fence()
        GROUPS = [(0, 8), (8, 8), (16, 6)]
        NWG, NWD = 2, 2
        wgu = [A.alloc([8, 2, 256], BF16) for _ in range(NWG)]; b_wgu = [nb() for _ in range(NWG)]
        l_wgu = [P.lane(f"wgu{li}_{i}") for i in range(NWG)]
        wd = [A.alloc([8, D], BF16) for _ in range(NWD)]; b_wd = [nb() for _ in range(NWD)]
        l_wd = [P.lane(f"wd{li}_{i}") for i in range(NWD)]
        hT = A.alloc([8, T], BF16); b_h = [[nb() for _ in range(4)] for _ in range(8)]
        sg = [A.alloc([512], F32) for _ in range(2)]; b_sg = [nb(), nb()]
        nexp = 8 if moe else 1
        if "ffn_all" in self.skip:
            nexp = 0
        si = 0
        gi = 0
        pgu = 0
        pdn = 0
        for e in range(nexp):
            gu_src = (self.mgu_d[e] if moe else self.fgu_d).rearrange("(c p) n -> p c n", p=128)
            dn_src = self.mdn_d[e] if moe else self.fdn_d
            for (g0, G) in GROUPS:
                ws = gi % NWD
                self.LOAD("pool", l_wd[ws], wd[ws][:, 0:G, :], dn_src[g0 * 128:(g0 + G) * 128, :].rearrange("(j p) n -> p j n", p=128), [b_wd[ws]])
                for sl_ in range(G // 2):
                    k = si % NWG
                    hc0 = g0 + 2 * sl_
                    self.LOAD("pool", l_wgu[k], wgu[k][:, :, 0, :], gu_src[:, :, hc0 * 128:hc0 * 128 + 256], [b_wgu[k]])
                    self.LOAD("pool", l_wgu[k], wgu[k][:, :, 1, :], gu_src[:, :, DFF + hc0 * 128:DFF + hc0 * 128 + 256], [b_wgu[k]])
                    for j in range(2):
                        hl = 2 * sl_ + j
                        for tb in range(4):
                            bg = (pgu % 2) * 2
                            pgu += 1
                            pg, pu = self.bank(bg), self.bank(bg + 1)
                            rd = [b_wgu[k]] + b_nT2[tb * 4:(tb + 1) * 4]
                            for c in range(8):
                                self.MM(pg, wgu[k][:, c, 0, j * 128:(j + 1) * 128], nT2[:, c, tb * 512:(tb + 1) * 512], rd, self.bankb(bg), start=(c == 0), stop=(c == 7))
                            for c in range(8):
                                self.MM(pu, wgu[k][:, c, 1, j * 128:(j + 1) * 128], nT2[:, c, tb * 512:(tb + 1) * 512], rd, self.bankb(bg + 1), start=(c == 0), stop=(c == 7))
                            q = pgu % 2
                            self.ACT(sg[q], pg, AF.Silu, self.bankb(bg), [b_sg[q]])
                            self.TT("dve", hT[:, hl, tb * 512:(tb + 1) * 512], sg[q], pu, ALU.mult, [b_sg[q]] + self.bankb(bg + 1), [b_h[hl][tb]])
                    si += 1
                for tt in range((1 if "ffn_dn1" in self.skip else NT) if "ffn_dn" not in self.skip else 0):
                    for half in range(2):
                        bd = 4 + pdn % 4
                        pdn += 1
                        pd = self.bank(bd)
                        for hl in range(G):
                            self.MM(pd, hT[:, hl, tt * 128:(tt + 1) * 128], wd[ws][:, hl, half * 512:(half + 1) * 512], [b_h[hl][tt // 4], b_wd[ws]], self.bankb(bd), start=(hl == 0), stop=(hl == G - 1))
                        xs = self.X[:, tt, half * 512:(half + 1) * 512]
                        if "ffn_noacc" in self.skip:
                            continue
                        if moe:
                            self.STT("dve", xs, pd, comb[:, tt, e:e + 1], xs, ALU.mult, ALU.add, self.bankb(bd) + [b_comb[tt], self.b_X[tt]], [self.b_X[tt]])
                        else:
                            self.TT("dve", xs, pd, xs, ALU.add, self.bankb(bd) + [self.b_X[tt]], [self.b_X[tt]])
                gi += 1
        self.dump("X0b" if li == 0 else "X1b", self.X, [128, NT, D], self.b_X)
        A.release()

    def layer1_mixer(self):
        A, P = self.A, self.P
        fence = P.fence()
        nb = lambda n=None: P.buf(n, after=fence)
        A.mark()
        kT = A.alloc([4, 256], BF16); Vm = A.alloc([2, 256], BF16); b_kv = nb("kv1")
        A.mark()
        wkv = A.alloc([8, 512], BF16); b_wkv = nb("wkv1")
        self.kv_prep(1, kT, Vm, b_kv, wkv, b_wkv, P.lane("wkv1"))
        A.release()
        fence = P.fence()
        win = A.alloc([8, 1792], BF16); b_win = nb()
        wout = A.alloc([8, D], BF16); b_wout = nb()
        gb = A.alloc([D], F32); lnG = A.alloc([MIX], F32); lnB = A.alloc([MIX], F32); b_gb = nb()
        wsT = A.alloc([12, 128], BF16); wsF = A.alloc([12, 128], F32); b_ws = nb()
        bfm = A.alloc([6, 128], F32); b_bfm = nb()
        ll = [P.lane(f"l1w{i}") for i in range(5)]
        self.LOAD("sp", ll[0], gb, self.grow_d[3, :].partition_broadcast(128), [b_gb])
        self.LOAD("sp", ll[0], lnG, self.lnr_d[0, :].partition_broadcast(128), [b_gb])
        self.LOAD("sp", ll[0], lnB, self.lnr_d[1, :].partition_broadcast(128), [b_gb])
        self.LOAD("pool", ll[1], win, self.win1_d.rearrange("(c p) n -> p c n", p=128), [b_win])
        self.LOAD("pool", ll[2], wout, self.wout_d[1].rearrange("(c p) n -> p c n", p=128), [b_wout])
        self.LOAD("sp", ll[3], wsF, self.wsT_d[:, :, :], [b_ws])
        self.TT("dve", wsT, wsF, self.mAT[:, 128:256].unsqueeze(1).to_broadcast([128, 12, 128]), ALU.mult, [b_ws, self.bc], [b_ws])
        bsv = self.bs_d.rearrange("(c h) t -> h c t", h=2)
        for hh in range(2):
            self.LOAD("sp", ll[4], bfm[hh * 64:(hh + 1) * 64, :, :], bsv[hh].partition_broadcast(64), [b_bfm])
        xn = [A.alloc([D], BF16) for _ in range(2)]; b_xn = [nb(), nb()]
        sq = A.alloc([D], BF16); stt = [A.alloc([8], F32) for _ in range(2)]; b_t = [nb(), nb()]
        nTt = [A.alloc([8, 128], BF16) for _ in range(2)]; b_nTt = [nb(), nb()]
        uT = [A.alloc([6, 128], F32) for _ in range(2)]; b_uT = [nb(), nb()]
        qTt = [A.alloc([2, 128], BF16) for _ in range(2)]; b_qTt = [nb(), nb()]
        vg = [A.alloc([MIX], F32) for _ in range(2)]; b_vg = [nb(), nb()]
        vtok = [A.alloc([MIX], BF16) for _ in range(2)]; b_vtok = [nb(), nb()]
        mixf = [A.alloc([6, 128], F32) for _ in range(2)]; b_mixf = [nb(), nb()]
        mixTt = [A.alloc([8, 128], BF16) for _ in range(2)]; b_mixy = [nb(), nb()]; b_mixo = [nb(), nb()]
        AS = self.attn_scratch()
        def stageA(tt):
            k = tt % 2
            B0 = 4 * k
            xs = self.X[:, tt, :]
            self.rms_tile(xs, self.b_X[tt], gb, b_gb, xn[k], b_xn[k], sq, stt[k], b_t[k])
            yield
            pb = self.bank(B0 + 2).bitcast(BF16)
            for c in range(8):
                self.TR(pb[:, c * 128:(c + 1) * 128], xn[k][:, c * 128:(c + 1) * 128], self.identb, [b_xn[k], self.bc], self.bankb(B0 + 2))
            self.CP("act", nTt[k], pb.rearrange("p (c t) -> p c t", c=8), self.bankb(B0 + 2), [b_nTt[k]])
            yield
            p1, p2 = self.bank(B0), self.bank(B0 + 1)
            for fc in range(6):
                dst = p1[:, fc * 128:(fc + 1) * 128] if fc < 4 else p2[:, (fc - 4) * 128:(fc - 3) * 128]
                bb = self.bankb(B0) if fc < 4 else [self.psb[2 * B0 + 2]]
                for c in range(8):
                    self.MM(dst, win[:, c, fc * 128:(fc + 1) * 128], nTt[k][:, c, :], [b_win, b_nTt[k]], bb, start=(c == 0), stop=(c == 7))
            for j in range(2):
                for c in range(8):
                    self.MM(p2[:, 256 + j * 128:256 + (j + 1) * 128], win[:, c, 1536 + j * 128:1536 + (j + 1) * 128], nTt[k][:, c, :], [b_win, b_nTt[k]], [self.psb[2 * B0 + 3]], start=(c == 0), stop=(c == 7))
            uflat = uT[k].rearrange("p c t -> p (c t)")
            self.ACT(uflat, self.ps[:, B0 * 512:B0 * 512 + 768], AF.Gelu, self.bankb(B0) + [self.psb[2 * B0 + 2]], [b_uT[k]])
            self.CP("dve", qTt[k], p2[:, 256:512].rearrange("p (c t) -> p c t", c=2), [self.psb[2 * B0 + 3]], [b_qTt[k]])
            yield
            p3, p4 = self.bank(B0 + 3), self.bank(B0 + 2)
            for c in range(8):
                self.MM(p3, nTt[k][:, c, :], win[:, c, 768:1280], [b_win, b_nTt[k]], self.bankb(B0 + 3), start=(c == 0), stop=(c == 7))
            for c in range(8):
                self.MM(p4[:, 0:256], nTt[k][:, c, :], win[:, c, 1280:1536], [b_win, b_nTt[k]], [self.psb[2 * B0 + 4]], start=(c == 0), stop=(c == 7))
            st_ = stt[k]
            self.ACT(vg[k][:, 0:512], p3, AF.Gelu, self.bankb(B0 + 3), [b_vg[k]], accum=st_[:, 3:4])
            self.ACT(vg[k][:, 512:768], p4[:, 0:256], AF.Gelu, [self.psb[2 * B0 + 4]], [b_vg[k]], accum=st_[:, 4:5])
            yield
            self.TT("dve", st_[:, 5:6], st_[:, 3:4], st_[:, 4:5], ALU.add, [b_vg[k]], [b_vg[k]])
            self.TS("dve", st_[:, 5:6], st_[:, 5:6], 1.0 / MIX, None, ALU.mult, None, [b_vg[k]], [b_vg[k]])
            self.TS("dve", vg[k], vg[k], st_[:, 5:6], None, ALU.subtract, None, [b_vg[k]], [b_vg[k]])
            yield
            self.ACT(sq[:, 0:MIX], vg[k], AF.Square, [b_vg[k]], [b_vg[k]], accum=st_[:, 6:7])
            yield
            self.ACT(st_[:, 7:8], st_[:, 6:7], AF.Sqrt, [b_vg[k], self.bc], [b_vg[k]], bias=self.eps(2), scale=1.0 / MIX)
            self.RECIP(st_[:, 7:8], st_[:, 7:8], [b_vg[k]], [b_vg[k]])
            yield
            self.STT("dve", vg[k], vg[k], st_[:, 7:8], lnG, ALU.mult, ALU.mult, [b_vg[k], b_gb], [b_vg[k]])
            self.TT("dve", vtok[k], vg[k], lnB, ALU.add, [b_vg[k], b_gb], [b_vtok[k]])

        def stageB(tt):
            k = tt % 2
            B0 = 4 * k
            p5, p6 = self.bank(B0), self.bank(B0 + 1)
            for g in range(12):
                cc, hp = g // 2, (g % 2) * 64
                dst = p5[hp:hp + 64, cc * 128:(cc + 1) * 128] if cc < 4 else p6[hp:hp + 64, (cc - 4) * 128:(cc - 3) * 128]
                bb = self.bankb(B0) if cc < 4 else [self.psb[2 * B0 + 2]]
                self.MM(dst, vtok[k][:, g * 64:(g + 1) * 64], wsT[:, g, :], [b_vtok[k], b_ws], bb)
            self.TT("dve", mixf[k][:, 0:4, :], p5.rearrange("p (c t) -> p c t", c=4), bfm[:, 0:4, :], ALU.add, self.bankb(B0) + [b_bfm], [b_mixf[k]])
            self.TT("dve", mixf[k][:, 4:6, :], p6[:, 0:256].rearrange("p (c t) -> p c t", c=2), bfm[:, 4:6, :], ALU.add, [self.psb[2 * B0 + 2], b_bfm], [b_mixf[k]])
            yield
            self.TT("pool", mixTt[k][:, 0:6, :], mixf[k], uT[k], ALU.mult, [b_mixf[k], b_uT[k]], [b_mixy[k]])
            yield
            self.attention_tile(qTt[k], b_qTt[k], slice(0, 128), kT, Vm, b_kv, mixTt[k][:, 6:8, :], b_mixo[k], (B0 + 2, B0 + 3), AS)
            yield
            for half in range(2):
                b = B0 + half
                pw = self.bank(b)
                for c in range(8):
                    self.MM(pw, mixTt[k][:, c, :], wout[:, c, half * 512:(half + 1) * 512], [b_mixy[k], b_mixo[k], b_wout], self.bankb(b), start=(c == 0), stop=(c == 7))
                xh = self.X[:, tt, half * 512:(half + 1) * 512]
                self.TT("dve", xh, pw, xh, ALU.add, self.bankb(b) + [self.b_X[tt]], [self.b_X[tt]])
        for i in range(NT + 1):
            gl = []
            if i >= 1:
                gl.append(stageB(i - 1))
            if i < NT:
                gl.append(stageA(i))
            drive(gl, 2)
        self.dump("X1a", self.X, [128, NT, D], self.b_X)
        A.release()

_CACHE = {}


def _prep_inputs(inp):
    f = lambda a: np.ascontiguousarray(a, dtype=np.float32)
    pcols = np.zeros((128, 64), np.float32)
    pcols[:, 0:20] = inp["rwkv_mu"][0].reshape(20, 128).T
    for i, k in enumerate(["rwkv_w0", "rwkv_a0", "rwkv_k_k", "rwkv_k_a", "rwkv_r_k", "rwkv_lnx_w", "rwkv_lnx_b"]):
        pcols[:, 20 + 6 * i:26 + 6 * i] = inp[k][0].reshape(6, 128).T
    grows = np.stack([inp["mem_norm_g"], inp["norm1_g"][0], inp["norm2_g"][0], inp["norm1_g"][1], inp["norm2_g"][1], inp["final_norm_g"]]).astype(np.float32)
    shared = {
        "grows": f(grows), "pcols": pcols,
        "rwkv_w_in": f(inp["rwkv_w_in"][0]), "rwkv_w2": f(inp["rwkv_w2"][0]), "rwkv_a2": f(inp["rwkv_a2"][0]), "rwkv_g2": f(inp["rwkv_g2"][0]),
        "w_kv_mem": f(inp["w_kv_mem"]), "w_out": f(inp["w_out"]),
        "ffn_w_gu": f(inp["ffn_w_gu"][0]), "ffn_w_down": f(inp["ffn_w_down"][0]),
        "gmlp_w_in": f(inp["gmlp_w_in"][0]),
        "gmlp_ln": f(np.stack([inp["gmlp_v_ln_g"][0], inp["gmlp_v_ln_b"][0]])),
        "gmlp_wsT": f(np.transpose(inp["gmlp_w_s"][0], (2, 0, 1))),
        "gmlp_bs": f(inp["gmlp_b_s"][0]),
        "moe_router": f(inp["moe_router"][0]), "moe_w_gu": f(inp["moe_w_gu"][0]), "moe_w_down": f(inp["moe_w_down"][0]),
    }
    return shared


def run(inp, cores=8, dbg=(), stage=99):
    key = (tuple(dbg), stage)
    if key not in _CACHE:
        k = K(dbg=dbg, stage=stage)
        k.build()
        _CACHE[key] = k
    k = _CACHE[key]
    shared = _prep_inputs(inp)
    in_maps = []
    for b in range(cores):
        m = dict(shared)
        m["x"] = np.ascontiguousarray(inp["x"][b], dtype=np.float32)
        m["mem"] = np.ascontiguousarray(inp["mem"][b], dtype=np.float32)
        in_maps.append(m)
    res = run_bass_kernel_spmd(k.nc, in_maps, core_ids=list(range(cores)))
    return res, k


def kernel(**inputs):
    res, k = run(inputs)
    out = np.stack([np.asarray(r["y"], dtype=np.float32) for r in res.results], axis=0)
    return out
```
